# Optimizing a Trainium2 kernel written in Bass

```python
import jax, jax.numpy as jnp
from jax import lax
import numpy as np

D_MODEL = 2048
BATCH = 8
SEQ = 4096
DEPTH = 2

HEAD_DIM = 64
MOBA_HEADS = 8
MOBA_BLOCK = 256
MOBA_TOPK = 3
MOBA_QCHUNK = 32
SB_HEADS = 8
SB_QBLOCK = 128
SWA_HEADS = 8
SWA_KV_HEADS = 2
SWA_WINDOW = 128
CONV_CH = 512
CONV_K = 3
D_FF = 5632
N_BRANCH = 4
EPS = 1e-6

MOBA_W = MOBA_HEADS * HEAD_DIM
SB_W = SB_HEADS * HEAD_DIM
SWA_QW = SWA_HEADS * HEAD_DIM
SWA_KVW = SWA_KV_HEADS * HEAD_DIM
BRANCH_W = 512
GATE_W = N_BRANCH * D_MODEL
IN_COLS = 3 * MOBA_W + 3 * SB_W + SWA_QW + 2 * SWA_KVW + 3 * CONV_CH + GATE_W

kernel_name = "hybrid_parallel_moba_stickbreak_swa_conv_macaron"


def _in_proj_splits():
    widths = [MOBA_W, MOBA_W, MOBA_W, SB_W, SB_W, SB_W, SWA_QW, SWA_KVW, SWA_KVW,
              CONV_CH, CONV_CH, CONV_CH]
    return [int(v) for v in np.cumsum(widths)]


def rms_norm(x, g):
    xf = x.astype(jnp.float32)
    y = xf * lax.rsqrt(jnp.mean(xf * xf, axis=-1, keepdims=True) + EPS)
    return (y * g.astype(jnp.float32)).astype(x.dtype)


def swiglu(x, w1, w3, w2):
    return (jax.nn.silu(x @ w1) * (x @ w3)) @ w2


def alibi_slopes(n):
    return 2.0 ** (-8.0 * jnp.arange(1, n + 1, dtype=jnp.float32) / n)


def moba_attention(q, k, v, slopes):
    bsz, seq, nh, dh = q.shape
    L = MOBA_BLOCK
    nb = -(-seq // L)
    pad = nb * L - seq
    n_sel = min(MOBA_TOPK, nb)
    qcl = MOBA_QCHUNK
    n_chunks = seq // qcl
    scale = dh ** -0.5
    q = q.transpose(0, 2, 1, 3)
    k = jnp.pad(k.transpose(0, 2, 1, 3), ((0, 0), (0, 0), (0, pad), (0, 0)))
    v = jnp.pad(v.transpose(0, 2, 1, 3), ((0, 0), (0, 0), (0, pad), (0, 0)))
    k_blocks = k.reshape(bsz, nh, nb, L, dh)
    v_blocks = v.reshape(bsz, nh, nb, L, dh)
    k_mean = jnp.mean(k_blocks.astype(jnp.float32), axis=3)
    gate = jnp.einsum('bhsd,bhnd->bhsn', q.astype(jnp.float32), k_mean)
    q_block = jnp.arange(seq) // L
    fully_past = jnp.arange(nb)[None, :] < q_block[:, None]
    gate = jnp.where(fully_past, gate, -jnp.inf)
    _, sel = lax.top_k(gate, n_sel)
    q_chunks = q.reshape(bsz, nh, n_chunks, qcl, dh).transpose(2, 0, 1, 3, 4)
    sel_chunks = sel.reshape(bsz, nh, n_chunks, qcl, n_sel).transpose(2, 0, 1, 3, 4)
    gather_blocks = jax.vmap(jax.vmap(lambda blocks, idx: blocks[idx]))

    def attend_chunk(args):
        c, q_c, sel_c = args
        t = c * qcl + jnp.arange(qcl)
        own = (c * qcl) // L
        k_own = lax.dynamic_slice_in_dim(k, own * L, L, axis=2)
        v_own = lax.dynamic_slice_in_dim(v, own * L, L, axis=2)
        d_own = t[:, None] - (own * L + jnp.arange(L))[None, :]
        lg_own = (jnp.einsum('bhqd,bhkd->bhqk', q_c, k_own,
                             preferred_element_type=jnp.float32) * scale
                  - slopes[:, None, None] * d_own)
        lg_own = jnp.where(d_own >= 0, lg_own, -jnp.inf)
        k_sel = gather_blocks(k_blocks, sel_c)
        v_sel = gather_blocks(v_blocks, sel_c)
        d_sel = t[:, None, None] - (sel_c[..., None] * L + jnp.arange(L))
        lg_sel = (jnp.einsum('bhqd,bhqrkd->bhqrk', q_c, k_sel,
                             preferred_element_type=jnp.float32) * scale
                  - slopes[:, None, None, None] * d_sel)
        keep = jnp.arange(n_sel) < own
        lg_sel = jnp.where(keep[:, None], lg_sel, -jnp.inf).reshape(bsz, nh, qcl, n_sel * L)
        p = jax.nn.softmax(jnp.concatenate([lg_sel, lg_own], axis=-1), axis=-1).astype(v.dtype)
        p_sel = p[..., :n_sel * L].reshape(bsz, nh, qcl, n_sel, L)
        p_own = p[..., n_sel * L:]
        return (jnp.einsum('bhqrk,bhqrkd->bhqd', p_sel, v_sel)
                + jnp.einsum('bhqk,bhkd->bhqd', p_own, v_own))

    out = lax.map(attend_chunk, (jnp.arange(n_chunks), q_chunks, sel_chunks))
    return out.transpose(1, 0, 3, 2, 4).reshape(bsz, seq, nh * dh)


def stick_breaking_attention(q, k, v):
    bsz, seq, nh, dh = q.shape
    scale = dh ** -0.5
    q = q.transpose(0, 2, 1, 3)
    k = k.transpose(0, 2, 1, 3)
    v = v.transpose(0, 2, 1, 3)
    outs = []
    for qb in range(seq // SB_QBLOCK):
        t0 = qb * SB_QBLOCK
        kl = t0 + SB_QBLOCK
        z = jnp.einsum('bhqd,bhkd->bhqk', q[:, :, t0:kl], k[:, :, :kl],
                       preferred_element_type=jnp.float32) * scale
        t = t0 + jnp.arange(SB_QBLOCK)
        causal = jnp.arange(kl)[None, :] < t[:, None]
        log_1m_beta = jnp.where(causal, jax.nn.log_sigmoid(-z), 0.0)
        after = lax.cumsum(log_1m_beta, axis=3, reverse=True) - log_1m_beta
        a = jnp.where(causal, jnp.exp(jax.nn.log_sigmoid(z) + after), 0.0)
        outs.append(jnp.einsum('bhqk,bhkd->bhqd', a.astype(v.dtype), v[:, :, :kl]))
    o = jnp.concatenate(outs, axis=2)
    return o.transpose(0, 2, 1, 3).reshape(bsz, seq, nh * dh)


def sliding_window_gqa(q, k, v, sinks, slopes):
    bsz, seq, nq, dh = q.shape
    nkv = k.shape[2]
    grp = nq // nkv
    W = SWA_WINDOW
    nblk = seq // W
    scale = dh ** -0.5
    qb = q.reshape(bsz, nblk, W, nkv, grp, dh)

    def with_prev(x):
        xb = x.reshape(bsz, nblk, W, nkv, dh)
        prev = jnp.pad(xb, ((0, 0), (1, 0), (0, 0), (0, 0), (0, 0)))[:, :-1]
        return jnp.concatenate([prev, xb], axis=2)

    kb = with_prev(k)
    vb = with_prev(v)
    dist = jnp.arange(W)[:, None] + W - jnp.arange(2 * W)[None, :]
    key_pos = (jnp.arange(nblk)[:, None] - 1) * W + jnp.arange(2 * W)[None, :]
    valid = (dist >= 0)[None] & (dist < W)[None] & (key_pos >= 0)[:, None, :]
    slope = slopes.reshape(nkv, grp)[None, None, :, :, None, None]
    logits = (jnp.einsum('bnqhgd,bnkhd->bnhgqk', qb, kb,
                         preferred_element_type=jnp.float32) * scale - slope * dist)
    logits = jnp.where(valid[None, :, None, None], logits, -jnp.inf)
    sink = sinks.astype(jnp.float32).reshape(nkv, grp)[None, None, :, :, None, None]
    m = jnp.maximum(jnp.max(logits, axis=-1, keepdims=True), sink)
    e = jnp.exp(logits - m)
    p = e / (jnp.sum(e, axis=-1, keepdims=True) + jnp.exp(sink - m))
    out = jnp.einsum('bnhgqk,bnkhd->bnqhgd', p.astype(v.dtype), vb)
    return out.reshape(bsz, seq, nq * dh)


def short_gated_conv(b_gate, c_gate, h, conv_w):
    u = c_gate * h
    y = lax.conv_general_dilated(u, conv_w, window_strides=(1,),
                                 padding=((CONV_K - 1, 0),),
                                 dimension_numbers=('NWC', 'WIO', 'NWC'),
                                 feature_group_count=u.shape[-1])
    return b_gate * y


def hybrid_mixer(xn, w_in, conv_w, sinks, w_branch, w_out):
    bsz, seq, _ = xn.shape
    proj = xn @ w_in
    (qa, ka, va, qb, kb, vb, qc, kc, vc, bg, cg, hd, gates) = jnp.split(
        proj, _in_proj_splits(), axis=-1)

    def heads(t, n):
        return t.reshape(bsz, seq, n, HEAD_DIM)

    slopes = alibi_slopes(SWA_HEADS + MOBA_HEADS)
    swa_slopes = slopes[:SWA_HEADS]
    moba_slopes = slopes[SWA_HEADS:]
    y_a = moba_attention(heads(qa, MOBA_HEADS), heads(ka, MOBA_HEADS),
                         heads(va, MOBA_HEADS), moba_slopes)
    y_b = stick_breaking_attention(heads(qb, SB_HEADS), heads(kb, SB_HEADS),
                                   heads(vb, SB_HEADS))
    y_c = sliding_window_gqa(heads(qc, SWA_HEADS), heads(kc, SWA_KV_HEADS),
                             heads(vc, SWA_KV_HEADS), sinks, swa_slopes)
    y_d = short_gated_conv(bg, cg, hd, conv_w)
    g = gates.reshape(bsz, seq, N_BRANCH, D_MODEL)
    branches = (y_a, y_b, y_c, y_d)
    merged = jax.nn.sigmoid(g[:, :, 0]) * (branches[0] @ w_branch[0])
    for n in range(1, N_BRANCH):
        merged = merged + jax.nn.sigmoid(g[:, :, n]) * (branches[n] @ w_branch[n])
    return merged @ w_out


def setup_inputs(seed: int = 0) -> dict:
    key = jax.random.key(seed)
    ks = jax.random.split(key, 16)

    def nrm(k, shape, scale):
        return jax.random.normal(k, shape, jnp.float32) * scale

    def gain(k, shape):
        return 1.0 + 0.02 * jax.random.normal(k, shape, jnp.float32)

    return {
        "x": nrm(ks[0], (BATCH, SEQ, D_MODEL), 1.0),
        "ffn1_norm": gain(ks[1], (DEPTH, D_MODEL)),
        "ffn1_w1": nrm(ks[2], (DEPTH, D_MODEL, D_FF), D_MODEL ** -0.5),
        "ffn1_w3": nrm(ks[3], (DEPTH, D_MODEL, D_FF), D_MODEL ** -0.5),
        "ffn1_w2": nrm(ks[4], (DEPTH, D_FF, D_MODEL), D_FF ** -0.5),
        "mix_norm": gain(ks[5], (DEPTH, D_MODEL)),
        "w_in": nrm(ks[6], (DEPTH, D_MODEL, IN_COLS), D_MODEL ** -0.5),
        "conv_w": nrm(ks[7], (DEPTH, CONV_K, 1, CONV_CH), CONV_K ** -0.5),
        "attn_sinks": nrm(ks[8], (DEPTH, SWA_HEADS), 1.0),
        "w_branch": nrm(ks[9], (DEPTH, N_BRANCH, BRANCH_W, D_MODEL), BRANCH_W ** -0.5),
        "w_out": nrm(ks[10], (DEPTH, D_MODEL, D_MODEL), D_MODEL ** -0.5),
        "ffn2_norm": gain(ks[11], (DEPTH, D_MODEL)),
        "ffn2_w1": nrm(ks[12], (DEPTH, D_MODEL, D_FF), D_MODEL ** -0.5),
        "ffn2_w3": nrm(ks[13], (DEPTH, D_MODEL, D_FF), D_MODEL ** -0.5),
        "ffn2_w2": nrm(ks[14], (DEPTH, D_FF, D_MODEL), D_FF ** -0.5),
        "final_norm": gain(ks[15], (D_MODEL,)),
    }


def reference(x, ffn1_norm, ffn1_w1, ffn1_w3, ffn1_w2, mix_norm, w_in, conv_w,
              attn_sinks, w_branch, w_out, ffn2_norm, ffn2_w1, ffn2_w3, ffn2_w2,
              final_norm):
    h = x
    for l in range(DEPTH):
        h = h + 0.5 * swiglu(rms_norm(h, ffn1_norm[l]), ffn1_w1[l], ffn1_w3[l], ffn1_w2[l])
        h = h + hybrid_mixer(rms_norm(h, mix_norm[l]), w_in[l], conv_w[l],
                             attn_sinks[l], w_branch[l], w_out[l])
        h = h + 0.5 * swiglu(rms_norm(h, ffn2_norm[l]), ffn2_w1[l], ffn2_w3[l], ffn2_w2[l])
    return rms_norm(h, final_norm)
```

```python
import numpy as np
import concourse.bass as bass
import concourse.mybir as mybir
from concourse.bass_utils import run_bass_kernel_spmd

F32 = mybir.dt.float32
BF16 = mybir.dt.bfloat16
AF = mybir.ActivationFunctionType
ALU = mybir.AluOpType
AX = mybir.AxisListType

D = 2048
DFF = 5632
KC = D // 128
FC = DFF // 128
EPS = 1e-6
NEG = -32768.0
O_QA, O_KA, O_VA, O_QB, O_KB, O_VB, O_QC, O_KC, O_VC, O_BG, O_CG, O_HD, O_GATE = (
    0, 512, 1024, 1536, 2048, 2560, 3072, 3584, 3712, 3840, 4352, 4864, 5376)
IN_COLS = 13568
R_QA, R_KA, R_QB, R_KB, R_QC, R_KC, R_BG, R_CG, R_HD = 0, 512, 1024, 1536, 2048, 2560, 2688, 3200, 3712
FEAT_ROWS = 4224
FM_SEGS = [(O_QA, R_QA, 512), (O_KA, R_KA, 512), (O_QB, R_QB, 512), (O_KB, R_KB, 512), (O_QC, R_QC, 512),
           (O_KC, R_KC, 128), (O_BG, R_BG, 512), (O_CG, R_CG, 512), (O_HD, R_HD, 512)]
VW = 1152


class Src:
    def __init__(self, name, k, inc):
        self.name, self.k, self.inc = name, k, inc
        self.count = 0
        self.sems = None


class Stream:
    def __init__(self, name, inorder=False):
        self.name = name
        self.ops = []
        self.seen = {}
        self.inorder = inorder
        self.src = Src(name, 1, 1)


class Buf:
    __slots__ = ("w", "r", "name")

    def __init__(self, name=""):
        self.w = {}
        self.r = {}
        self.name = name


class Sched:
    def __init__(self):
        self.pe = Stream("pe", inorder=True)
        self.act = Stream("act")
        self.dve = Stream("dve")
        self.pool = Stream("pool")
        self.sp = Stream("sp")
        self.streams = [self.pe, self.act, self.dve, self.pool, self.sp]
        self.q_sp = Src("qsp", 12, 16)
        self.q_pool = Src("qpool", 8, 16)
        self.srcs = [s.src for s in self.streams] + [self.q_sp, self.q_pool]

    def _wait(self, st, ev):
        src, slot, val = ev
        if src is st.src and st.inorder:
            return
        key = (id(src), slot)
        if st.seen.get(key, 0) < val:
            st.ops.append(("w", src, slot, val))
            st.seen[key] = val

    def emit(self, st, fn, reads=(), writes=(), q=None):
        deps = []
        for b in reads:
            deps.extend((s, sl, v) for (s, sl), v in b.w.values())
        for b in writes:
            deps.extend((s, sl, v) for (s, sl), v in b.w.values())
            deps.extend((s, sl, v) for (s, sl), v in b.r.values())
        for ev in deps:
            self._wait(st, ev)
        src = q if q is not None else st.src
        if src.k > 1:
            i = src.count
            slot = i % src.k
            val = 16 * (i // src.k + 1)
            if i >= src.k:
                self._wait(st, (src, slot, val - 16))
            src.count += 1
        else:
            src.count += 1
            slot, val = 0, src.count
        st.ops.append(("op", fn, src, slot))
        ev = (src, slot, val)
        for b in reads:
            key = (id(src), slot)
            old = b.r.get(key)
            if old is None or old[1] < val:
                b.r[key] = ((src, slot), val)
        for b in writes:
            b.w[(id(src), slot)] = ((src, slot), val)
            b.r = {}
        return ev

    def barrier(self):
        evs = []
        for src in self.srcs:
            if src.count == 0:
                continue
            if src.k == 1:
                evs.append((src, 0, src.count))
            else:
                for slot in range(min(src.k, src.count)):
                    i_last = ((src.count - 1 - slot) // src.k) * src.k + slot
                    evs.append((src, slot, 16 * (i_last // src.k + 1)))
        for st in self.streams:
            for ev in evs:
                self._wait(st, ev)

    def replay(self, st, eng):
        for op in st.ops:
            if op[0] == "w":
                _, src, slot, val = op
                eng.wait_ge(src.sems[slot], val)
            else:
                _, fn, src, slot = op
                fn(eng).then_inc(src.sems[slot], src.inc)


def _bf(x):
    import ml_dtypes
    return np.asarray(x, np.float32).astype(ml_dtypes.bfloat16).astype(np.float32)


def make_consts(S):
    nqt = S // 128
    j = np.arange(128)[:, None]
    i = np.arange(128)[None, :]
    c = {}
    c["c_ident"] = np.eye(128, dtype=np.float32)
    c["c_mstrict"] = np.where(j < i, 0.0, NEG).astype(np.float32)
    c["c_mincl"] = np.where(j <= i, 0.0, NEG).astype(np.float32)
    c["c_mpair"] = np.concatenate([np.where(j > i, 0.0, NEG), np.where(j <= i, 0.0, NEG)], axis=1).astype(np.float32)
    c["c_negu"] = np.where(j >= i, -8.0, 0.0).astype(np.float32)
    ind = np.zeros((128, 160), np.float32)
    ind[:, 31] = 1.0
    c["c_ind"] = ind
    nep = np.zeros((128, 32, 128), np.float32)
    for ks in range(32):
        nep[ks + 1:32, ks, :] = -8.0
    c["c_negep"] = nep.reshape(128, 4096)
    onesp = np.zeros((128, 256), np.float32)
    onesp[:, 0:64] = 1.0
    onesp[:, 128 + 64:256] = 1.0
    c["c_onesp"] = onesp
    c["c_neg8i"] = (-8.0 * np.eye(128)).astype(np.float32)
    c["c_ones128"] = np.ones((128, 128), np.float32)
    tq = np.arange(32)[:, None]
    n = np.arange(16)[None, :]
    past = (n < (tq // 2))
    c["c_pastneg"] = np.broadcast_to(np.where(past, 0.0, -1e30).reshape(1, 512), (128, 512)).astype(np.float32).copy()
    c["c_negbig"] = np.broadcast_to(np.where(past, NEG, 0.0).reshape(1, 512), (128, 512)).astype(np.float32).copy()
    slopes = 2.0 ** (-8.0 * np.arange(1, 17, dtype=np.float64) / 16.0)
    t = np.arange(S)
    ti, tt = (t % 128).astype(np.float32), ((t // 128) * 128).astype(np.float32)
    blk = t // 256

    def aug(sl, with_sel):
        s8 = 8.0 * float(_bf(sl))
        q = np.stack([ti, tt, np.full(S, s8, np.float32), np.full(S, s8, np.float32)]).astype(np.float32)
        k = np.stack([np.full(S, -s8, np.float32), np.full(S, -s8, np.float32), ti, tt]).astype(np.float32)
        if with_sel:
            sel = (blk[None, :] == np.arange(16)[:, None]).astype(np.float32)
            k = np.concatenate([sel, k], axis=0)
        return q, k

    qa, ka, qc, kc = [], [], [], []
    for h in range(8):
        q, k = aug(slopes[8 + h], True)
        qa.append(q); ka.append(k)
        q, k = aug(slopes[h], False)
        qc.append(q); kc.append(k)
    c["c_qaug_a"] = np.stack(qa)
    c["c_kaug_a"] = np.stack(ka)
    c["c_qaug_c"] = np.stack(qc)
    c["c_kaug_c"] = np.stack(kc)
    return c


WNAMES = ["ffn1_w1", "ffn1_w3", "ffn1_w2", "w_in", "w_branch", "w_out", "ffn2_w1", "ffn2_w3", "ffn2_w2"]
WSHAPES = {"ffn1_w1": (D, DFF), "ffn1_w3": (D, DFF), "ffn1_w2": (DFF, D), "w_in": (D, IN_COLS),
           "w_branch": (4 * 512, D), "w_out": (D, D), "ffn2_w1": (D, DFF), "ffn2_w3": (D, DFF), "ffn2_w2": (DFF, D)}


NORM_IDX = {"ffn1_norm": 0, "mix_norm": 1, "ffn2_norm": 2}


def build(S=4096, L=2, debug=False):
    from contextlib import ExitStack
    assert S % 512 == 0
    NTG, NQT, NBLK = S // 512, S // 128, S // 256
    nc = bass.Bass("TRN2", target_bir_lowering=False)
    sc = Sched()
    consts = make_consts(S)

    def din(name, shape, dt=F32):
        return nc.dram_tensor(name, list(shape), dt, kind="ExternalInput").ap()

    x_in = din("x", [S, D])
    out_d = nc.dram_tensor("out", [S, D], F32, kind="ExternalOutput").ap()
    wsrc = {n: din(n, (L,) + WSHAPES[n]) for n in WNAMES}
    norms = {n: din(n, [L, D]) for n in ["ffn1_norm", "mix_norm", "ffn2_norm"]}
    fnorm = din("final_norm", [1, D])
    conv_w = din("conv_w", [L, 3, 512])
    sinks = din("attn_sinks", [L, 8])
    cin = {k: din(k, v.shape) for k, v in consts.items()}
    skind = "ExternalOutput" if debug else "Internal"
    wbf = {n: nc.dram_tensor("s_" + n, [L] + list(WSHAPES[n]), BF16, kind="Internal").ap() for n in WNAMES}
    hbuf = nc.dram_tensor("s_h", [S, D], F32, kind=skind).ap()
    featT = nc.dram_tensor("s_featT", [FEAT_ROWS, S], BF16, kind=skind).ap()
    vscr = nc.dram_tensor("s_v", [S, VW], BF16, kind=skind).ap()
    yT = nc.dram_tensor("s_yT", [D, S], BF16, kind=skind).ap()
    b_hbuf, b_featT, b_vscr, b_yT = Buf("hbuf"), Buf("featT"), Buf("vscr"), Buf("yT")
    if debug:
        d_gm = nc.dram_tensor("d_gm", [128, 512], F32, kind="ExternalOutput").ap()
        d_top8 = nc.dram_tensor("d_top8", [128, 256], F32, kind="ExternalOutput").ap()
        d_lt = nc.dram_tensor("d_lt", [128, 512], F32, kind="ExternalOutput").ap()
        d_km = nc.dram_tensor("d_km", [128, 16], F32, kind="ExternalOutput").ap()
        b_dbg = Buf("dbg")
    b_w = {n: [Buf(n + str(l)) for l in range(L)] for n in WNAMES}
    b_out = Buf("out")

    with ExitStack() as top:
        uid = [0]

        def sb(name, shape, dt, es=top):
            uid[0] += 1
            return es.enter_context(nc.sbuf_tensor("t%d_%s" % (uid[0], name), list(shape), dt))

        banks = [top.enter_context(nc.psum_tensor("bank%d" % i, [128, 512], F32)) for i in range(8)]
        b_bank = [Buf("bank%d" % i) for i in range(8)]
        for s_ in sc.srcs:
            s_.sems = [top.enter_context(nc.semaphore("%s_%d" % (s_.name, i))) for i in range(s_.k)]

        pe, act, dve, pool, sp = sc.pe, sc.act, sc.dve, sc.pool, sc.sp

        def dma_sp(out, in_, reads, writes):
            sc.emit(sp, lambda e: e.dma_start(out=out, in_=in_), reads, writes, q=sc.q_sp)

        def dma_pool(out, in_, reads, writes):
            sc.emit(pool, lambda e: e.dma_start(out=out, in_=in_), reads, writes, q=sc.q_pool)

        def mm(out, lhsT, rhs, start, stop, reads, writes):
            sc.emit(pe, lambda e: e.matmul(out, lhsT, rhs, start=start, stop=stop), reads, writes)

        b_const = Buf("const")
        ident = sb("c_ident", [128, 128], BF16)
        dma_pool(ident[:], cin["c_ident"][:], [], [b_const])
        gT = sb("gT", [128, 3 * L, KC], F32)
        for l in range(L):
            for n, ni in NORM_IDX.items():
                for k in range(KC):
                    dma_pool(gT[:, l * 3 + ni, k:k + 1],
                             norms[n][l, k * 128:(k + 1) * 128].rearrange("(c o) -> c o", o=1), [], [b_const])

        conv_tasks = []
        for l in range(L):
            for n in WNAMES:
                K_, N_ = WSHAPES[n]
                nch = -(-N_ // 1408)
                while N_ % nch:
                    nch += 1
                cw_ = N_ // nch
                for r0 in range(0, K_, 128):
                    for ci in range(nch):
                        conv_tasks.append((n, l, r0, ci * cw_, cw_))
        cvs = {"pos": 0, "pend": [], "bufs": None, "it": 0}

        def cv_open(es, nb=4):
            fin = [sb("cv_in%d" % i, [128, 1408], F32, es) for i in range(nb)]
            fout = [sb("cv_out%d" % i, [128, 1408], BF16, es) for i in range(nb)]
            cvs["bufs"] = (fin, fout, [Buf() for _ in range(nb)], [Buf() for _ in range(nb)], nb)

        def cv_flush():
            for (dst_ap, o_, bfo, bw_) in cvs["pend"]:
                dma_sp(dst_ap, o_, [bfo], [bw_])
            cvs["pend"] = []

        def pump(k, engines):
            fin, fout, b_fin, b_fout, nb = cvs["bufs"]
            for _ in range(k):
                if cvs["pos"] >= len(conv_tasks):
                    break
                n, l, r0, c0, cw = conv_tasks[cvs["pos"]]
                cvs["pos"] += 1
                i = cvs["it"] % nb
                cvs["it"] += 1
                src_ap = wsrc[n][l, r0:r0 + 128, c0:c0 + cw]
                dst_ap = wbf[n][l, r0:r0 + 128, c0:c0 + cw]
                dma_sp(fin[i][:, :cw], src_ap, [], [b_fin[i]])
                o_, a_ = fout[i][:, :cw], fin[i][:, :cw]
                eng = engines[cvs["it"] % len(engines)]
                if eng is act:
                    sc.emit(act, lambda e, o=o_, a=a_: e.copy(out=o, in_=a), [b_fin[i]], [b_fout[i]])
                else:
                    sc.emit(eng, lambda e, o=o_, a=a_: e.tensor_copy(out=o, in_=a), [b_fin[i]], [b_fout[i]])
                cvs["pend"].append((dst_ap, o_, b_fout[i], b_w[n][l]))
                while len(cvs["pend"]) > 2:
                    dst2, o2, bfo2, bw2 = cvs["pend"].pop(0)
                    dma_sp(dst2, o2, [bfo2], [bw2])

        def pump_until(names_layers, engines):
            need = set(names_layers)
            last = -1
            for idx, t in enumerate(conv_tasks):
                if (t[0], t[1]) in need:
                    last = idx
            if last >= cvs["pos"]:
                pump(last + 1 - cvs["pos"], engines)
            cv_flush()

        with ExitStack() as es:
            cv_open(es, 6)
            pump_until([("ffn1_w1", 0), ("ffn1_w3", 0), ("ffn1_w2", 0), ("w_in", 0)], [dve, act, pool])
        sc.barrier()

        def token_phase(first, last, l):
            with ExitStack() as es:
                h = sb("h", [128, 4, D], F32, es)
                b_h = [Buf("h%d" % s) for s in range(4)]
                xnT = sb("xnT", [128, KC, 512], BF16, es)
                b_xnT = Buf("xnT")
                hidT = sb("hidT", [128, FC, 512], BF16, es)
                b_hidT = Buf("hidT")
                NSL = 3
                slots = [sb("wslot%d" % i, [128, 16 * 512], BF16, es) for i in range(NSL)]
                b_slot = [Buf("slot%d" % i) for i in range(NSL)]
                xtok = sb("xtok", [128, D], BF16, es)
                b_xtok = Buf("xtok")
                ss = sb("ss", [128, 8], F32, es)
                b_ss = Buf("ss")
                sgt = [sb("sg%d" % i, [128, 512], F32, es) for i in range(2)]
                b_sg = [Buf() for _ in range(2)]
                if not first:
                    acc = sb("acc", [128, 4, 512], F32, es)
                    b_acc = [Buf() for _ in range(4)]
                    tmp = sb("tmpf", [128, 512], F32, es)
                    b_tmp = Buf("tmp")
                if not last:
                    stg = [sb("stg%d" % i, [128, 4, 512], BF16, es) for i in range(2)]
                    b_stg = [Buf() for _ in range(2)]
                    vst = [sb("vst%d" % i, [128, 512], BF16, es) for i in range(2)]
                    b_vst = [Buf() for _ in range(2)]
                else:
                    gbc = sb("gbc", [128, D], F32, es)
                    b_gbc = Buf("gbc")
                st = {"slot": 0, "bank": 0, "sg": 0, "stg": 0, "vst": 0}
                if last:
                    tgt = len(conv_tasks)
                elif first:
                    tgt = sum(1 for t in conv_tasks if t[1] == 0)
                else:
                    tgt = len(conv_tasks)
                todo = max(0, tgt - cvs["pos"])
                n_pts = NTG * (30 if first else 60)
                tp_per = -(-todo // max(1, n_pts - 20)) if todo > 0 else 0
                if todo > 0:
                    cv_open(es, 3)

                n_win0 = sum(1 for t in conv_tasks if t[1] == 0 and t[0] in WNAMES[:4])
                cur = {"tg": 0}

                def tp_pump():
                    if tp_per > 0 and cvs["pos"] < tgt:
                        k_ = tp_per
                        if first and cur["tg"] == 0 and cvs["pos"] < n_win0:
                            k_ = 12
                        pump(min(k_, tgt - cvs["pos"]), [pool, dve] if (first and cur["tg"] == 0) else [pool])

                def load_panel(W, bW, k0, kn, c0, C):
                    i = st["slot"] % NSL
                    st["slot"] += 1
                    view = slots[i][:, 0:kn * C].rearrange("p (k c) -> p k c", c=C)
                    src_ap = W[k0 * 128:(k0 + kn) * 128, c0:c0 + C].rearrange("(k p) c -> p k c", p=128)
                    dma_sp(view, src_ap, [bW], [b_slot[i]])
                    return view, b_slot[i]

                def rstd_for(s):
                    sc.emit(act, lambda e, s=s: e.activation(out=xtok[:], in_=h[:, s, :], func=AF.Square,
                                                             accum_out=ss[:, 0:1]), [b_h[s]], [b_xtok, b_ss])
                    sc.emit(act, lambda e: e.activation(out=ss[:, 1:2], in_=ss[:, 0:1], func=AF.Sqrt,
                                                        scale=1.0 / D, bias=ss[:, 4:5]), [b_ss], [b_ss])
                    sc.emit(dve, lambda e: e.reciprocal(out=ss[:, 2:3], in_=ss[:, 1:2]), [b_ss], [b_ss])

                sc.emit(dve, lambda e: e.memset(ss[:, 4:5], EPS), [], [b_ss])

                def norm_transpose(gi):
                    for s in range(4):
                        rstd_for(s)
                        sc.emit(dve, lambda e, s=s: e.tensor_scalar(
                            out=xtok[:], in0=h[:, s, :], scalar1=ss[:, 2:3], scalar2=None, op0=ALU.mult),
                            [b_h[s], b_ss], [b_xtok])
                        for j4 in range(4):
                            bk = 6 + (j4 % 2)
                            pv = banks[bk][:].bitcast(BF16)
                            for jj in range(4):
                                kc_ = j4 * 4 + jj
                                sc.emit(pe, lambda e, pv=pv, jj=jj, kc_=kc_: e.transpose(
                                    pv[:, jj * 128:(jj + 1) * 128], xtok[:, kc_ * 128:(kc_ + 1) * 128], ident[:]),
                                    [b_xtok, b_const], [b_bank[bk]])
                            for jj in range(4):
                                kc_ = j4 * 4 + jj
                                if jj % 2 == 0:
                                    sc.emit(act, lambda e, pv=pv, jj=jj, kc_=kc_, s=s: e.activation(
                                        out=xnT[:, kc_, s * 128:(s + 1) * 128], in_=pv[:, jj * 128:(jj + 1) * 128],
                                        func=AF.Copy, scale=gT[:, gi, kc_:kc_ + 1]),
                                        [b_bank[bk], b_const], [b_xnT])
                                else:
                                    sc.emit(dve, lambda e, pv=pv, jj=jj, kc_=kc_, s=s: e.tensor_scalar(
                                        out=xnT[:, kc_, s * 128:(s + 1) * 128], in0=pv[:, jj * 128:(jj + 1) * 128],
                                        scalar1=gT[:, gi, kc_:kc_ + 1], scalar2=None, op0=ALU.mult),
                                        [b_bank[bk], b_const], [b_xnT])

                def mm_tm(xT, bxT, kct, W, bW, ncols, scale, k_off=0):
                    kp = []
                    k0 = 0
                    while k0 < kct:
                        kn = min(16, kct - k0)
                        kp.append((k0, kn))
                        k0 += kn
                    for ci, c0 in enumerate(range(0, ncols, 512)):
                        tp_pump()
                        for (k0, kn) in kp:
                            pw, bpw = load_panel(W, bW, k0, kn, c0, 512)
                            for s in range(4):
                                bk = (ci % 2) * 4 + s
                                for k in range(kn):
                                    kk = k0 + k
                                    mm(banks[bk][:], xT[:, k_off + kk, s * 128:(s + 1) * 128], pw[:, k, :],
                                       kk == 0, kk == kct - 1, [bpw, bxT], [b_bank[bk]])
                        for s in range(4):
                            bk = (ci % 2) * 4 + s
                            sc.emit(dve, lambda e, s=s, c0=c0, bk=bk: e.scalar_tensor_tensor(
                                out=h[:, s, c0:c0 + 512], in0=banks[bk][:], scalar=float(scale),
                                in1=h[:, s, c0:c0 + 512], op0=ALU.mult, op1=ALU.add),
                                [b_bank[bk], b_h[s]], [b_h[s]])

                def ffn(l_, pre):
                    w1, bw1 = wbf[pre + "_w1"][l_], b_w[pre + "_w1"][l_]
                    w3, bw3 = wbf[pre + "_w3"][l_], b_w[pre + "_w3"][l_]
                    w2, bw2 = wbf[pre + "_w2"][l_], b_w[pre + "_w2"][l_]
                    for c0 in range(0, DFF, 512):
                        tp_pump()
                        p1, bp1 = load_panel(w1, bw1, 0, KC, c0, 512)
                        p3, bp3 = load_panel(w3, bw3, 0, KC, c0, 512)
                        for f in range(4):
                            fc = c0 // 128 + f
                            ba = st["bank"] % 2
                            st["bank"] += 1
                            b1, b3 = ba, 2 + ba
                            for k in range(KC):
                                mm(banks[b1][:], p1[:, k, f * 128:(f + 1) * 128], xnT[:, k, :], k == 0, k == KC - 1,
                                   [bp1, b_xnT], [b_bank[b1]])
                            for k in range(KC):
                                mm(banks[b3][:], p3[:, k, f * 128:(f + 1) * 128], xnT[:, k, :], k == 0, k == KC - 1,
                                   [bp3, b_xnT], [b_bank[b3]])
                            si = st["sg"] % 2
                            st["sg"] += 1
                            sc.emit(act, lambda e, si=si, b1=b1: e.activation(out=sgt[si][:], in_=banks[b1][:], func=AF.Silu),
                                    [b_bank[b1]], [b_sg[si]])
                            sc.emit(dve, lambda e, si=si, b3=b3, fc=fc: e.tensor_tensor(
                                out=hidT[:, fc, :], in0=banks[b3][:], in1=sgt[si][:], op=ALU.mult),
                                [b_bank[b3], b_sg[si]], [b_hidT])
                    mm_tm(hidT, b_hidT, FC, w2, bw2, D, 0.5)

                def inproj_a(l_, tg):
                    W, bW = wbf["w_in"][l_], b_w["w_in"][l_]
                    for (ocol, row, width) in FM_SEGS:
                        tp_pump()
                        pw, bpw = load_panel(W, bW, 0, KC, ocol, width)
                        si = st["stg"] % 2
                        st["stg"] += 1
                        nf = width // 128
                        for f in range(nf):
                            bk = st["bank"] % 4
                            st["bank"] += 1
                            for k in range(KC):
                                mm(banks[bk][:], pw[:, k, f * 128:(f + 1) * 128], xnT[:, k, :], k == 0, k == KC - 1,
                                   [bpw, b_xnT], [b_bank[bk]])
                            if f % 2 == 0:
                                sc.emit(act, lambda e, si=si, f=f, bk=bk: e.copy(out=stg[si][:, f, :], in_=banks[bk][:]),
                                        [b_bank[bk]], [b_stg[si]])
                            else:
                                sc.emit(dve, lambda e, si=si, f=f, bk=bk: e.tensor_copy(out=stg[si][:, f, :], in_=banks[bk][:]),
                                        [b_bank[bk]], [b_stg[si]])
                        dst = featT[row:row + width, tg * 512:(tg + 1) * 512].rearrange("(f p) t -> p f t", p=128)
                        dma_pool(dst, stg[si][:, 0:nf, :], [b_stg[si]], [b_featT])
                    for (ocol, vo, width) in [(O_VA, 0, 512), (O_VB, 512, 512), (O_VC, 1024, 128)]:
                        pw, bpw = load_panel(W, bW, 0, KC, ocol, width)
                        for s in range(4):
                            bk = 4 + s
                            for k in range(KC):
                                mm(banks[bk][:, 0:width], xnT[:, k, s * 128:(s + 1) * 128], pw[:, k, :], k == 0, k == KC - 1,
                                   [bpw, b_xnT], [b_bank[bk]])
                            vi = st["vst"] % 2
                            st["vst"] += 1
                            sc.emit(act, lambda e, vi=vi, bk=bk, width=width: e.copy(
                                out=vst[vi][:, 0:width], in_=banks[bk][:, 0:width]), [b_bank[bk]], [b_vst[vi]])
                            r0 = tg * 512 + s * 128
                            dma_pool(vscr[r0:r0 + 128, vo:vo + width], vst[vi][:, 0:width], [b_vst[vi]], [b_vscr])

                def mixer_post(l_, tg):
                    W, bW = wbf["w_in"][l_], b_w["w_in"][l_]
                    WB, bWB = wbf["w_branch"][l_], b_w["w_branch"][l_]
                    src_ap = yT[:, tg * 512:(tg + 1) * 512].rearrange("(k p) t -> p k t", p=128)
                    dma_pool(hidT[:, 0:16, :], src_ap, [b_yT], [b_hidT])
                    for c0 in range(0, D, 512):
                        for n in range(4):
                            tp_pump()
                            pw, bpw = load_panel(W, bW, 0, KC, O_GATE + n * D + c0, 512)
                            bp, bbp = load_panel(WB, bWB, n * 4, 4, c0, 512)
                            for f in range(4):
                                fc = c0 // 128 + f
                                ba = st["bank"] % 2
                                st["bank"] += 1
                                bg_, bb_ = ba, 2 + ba
                                for k in range(KC):
                                    mm(banks[bg_][:], pw[:, k, f * 128:(f + 1) * 128], xnT[:, k, :], k == 0, k == KC - 1,
                                       [bpw, b_xnT], [b_bank[bg_]])
                                for k in range(4):
                                    mm(banks[bb_][:], bp[:, k, f * 128:(f + 1) * 128], hidT[:, n * 4 + k, :],
                                       k == 0, k == 3, [bbp, b_hidT], [b_bank[bb_]])
                                si = st["sg"] % 2
                                st["sg"] += 1
                                sc.emit(act, lambda e, si=si, bg_=bg_: e.activation(out=sgt[si][:], in_=banks[bg_][:],
                                                                                   func=AF.Sigmoid),
                                        [b_bank[bg_]], [b_sg[si]])
                                if n == 0:
                                    sc.emit(dve, lambda e, si=si, bb_=bb_, f=f: e.tensor_tensor(
                                        out=acc[:, f, :], in0=banks[bb_][:], in1=sgt[si][:], op=ALU.mult),
                                        [b_bank[bb_], b_sg[si]], [b_acc[f]])
                                else:
                                    sc.emit(dve, lambda e, si=si, bb_=bb_: e.tensor_tensor(
                                        out=tmp[:], in0=banks[bb_][:], in1=sgt[si][:], op=ALU.mult),
                                        [b_bank[bb_], b_sg[si]], [b_tmp])
                                    if n < 3:
                                        sc.emit(pool, lambda e, f=f: e.tensor_tensor(out=acc[:, f, :], in0=acc[:, f, :],
                                                                                      in1=tmp[:], op=ALU.add),
                                                [b_acc[f], b_tmp], [b_acc[f]])
                                    else:
                                        sc.emit(pool, lambda e, f=f, fc=fc: e.tensor_tensor(
                                            out=hidT[:, 16 + fc, :], in0=acc[:, f, :], in1=tmp[:], op=ALU.add),
                                            [b_acc[f], b_tmp], [b_hidT])
                    mm_tm(hidT, b_hidT, KC, wbf["w_out"][l_], b_w["w_out"][l_], D, 1.0, k_off=16)

                for tg in range(NTG):
                    cur["tg"] = tg
                    r0 = tg * 512
                    srcd, bsrc = (x_in, None) if first else (hbuf, b_hbuf)
                    for s in range(4):
                        dma_pool(h[:, s, :], srcd[r0 + s * 128:r0 + (s + 1) * 128, :],
                                 [bsrc] if bsrc else [], [b_h[s]])
                    if not first:
                        norm_transpose((l - 1) * 3 + 1)
                        mixer_post(l - 1, tg)
                        norm_transpose((l - 1) * 3 + 2)
                        ffn(l - 1, "ffn2")
                    if not last:
                        norm_transpose(l * 3 + 0)
                        ffn(l, "ffn1")
                        for s in range(4):
                            dma_pool(hbuf[r0 + s * 128:r0 + (s + 1) * 128, :], h[:, s, :], [b_h[s]], [b_hbuf])
                        norm_transpose(l * 3 + 1)
                        if first and tg == 0 and cvs["pos"] < n_win0:
                            pump(n_win0 - cvs["pos"], [pool, dve])
                            cv_flush()
                        inproj_a(l, tg)
                    else:
                        if tg == 0:
                            dma_pool(gbc[:], fnorm[0, :].partition_broadcast(128), [], [b_gbc])
                        for s in range(4):
                            rstd_for(s)
                            sc.emit(dve, lambda e, s=s: e.scalar_tensor_tensor(
                                out=h[:, s, :], in0=h[:, s, :], scalar=ss[:, 2:3], in1=gbc[:],
                                op0=ALU.mult, op1=ALU.mult), [b_h[s], b_ss, b_gbc], [b_h[s]])
                            dma_pool(out_d[r0 + s * 128:r0 + (s + 1) * 128, :], h[:, s, :], [b_h[s]], [b_out])
                if todo > 0:
                    if cvs["pos"] < tgt:
                        pump(tgt - cvs["pos"], [pool, dve])
                    cv_flush()
            sc.barrier()

        def attention_phase(l):
            def load_feat(tile, buf, row, nrows=128, p0=0):
                dma_pool(tile[p0:p0 + nrows, :], featT[row:row + nrows, :], [b_featT], [buf])

            with ExitStack() as es:
                cvbs = [[sb("cvb%d_%d" % (j, i), [128, S], BF16, es) for i in range(3)] for j in range(2)]
                b_cvbs = [[Buf() for _ in range(3)] for _ in range(2)]
                cus = [sb("cu%d" % j, [128, S + 2], F32, es) for j in range(2)]
                cys = [sb("cy%d" % j, [128, S], F32, es) for j in range(2)]
                cyos = [sb("cyo%d" % j, [128, S], BF16, es) for j in range(2)]
                b_cus, b_cys, b_cyos = [Buf(), Buf()], [Buf(), Buf()], [Buf(), Buf()]
                cws = [sb("cw%d" % j, [128, 4], F32, es) for j in range(2)]
                b_cws = [Buf(), Buf()]

                def conv_load(ch):
                    j = ch % 2
                    load_feat(cvbs[j][0], b_cvbs[j][0], R_BG + ch * 128)
                    load_feat(cvbs[j][1], b_cvbs[j][1], R_CG + ch * 128)
                    load_feat(cvbs[j][2], b_cvbs[j][2], R_HD + ch * 128)
                    for k in range(3):
                        dma_pool(cws[j][:, k:k + 1], conv_w[l, k, ch * 128:(ch + 1) * 128].rearrange("(c o) -> c o", o=1),
                                 [], [b_cws[j]])

                conv_load(0)
                for ch in range(4):
                    if ch + 1 < 4:
                        conv_load(ch + 1)
                    j = ch % 2
                    cvb, b_cvb, cu, cy, cyo, cw = cvbs[j], b_cvbs[j], cus[j], cys[j], cyos[j], cws[j]
                    b_cu, b_cy, b_cyo, b_cw = b_cus[j], b_cys[j], b_cyos[j], b_cws[j]
                    sc.emit(dve, lambda e, cu=cu: e.memset(cu[:, 0:2], 0.0), [], [b_cu])
                    sc.emit(dve, lambda e, cu=cu, cvb=cvb: e.tensor_tensor(out=cu[:, 2:S + 2], in0=cvb[1][:], in1=cvb[2][:],
                                                                           op=ALU.mult), [b_cvb[1], b_cvb[2]], [b_cu])
                    sc.emit(dve, lambda e, cu=cu, cy=cy, cw=cw: e.tensor_scalar(
                        out=cy[:], in0=cu[:, 0:S], scalar1=cw[:, 0:1], scalar2=None, op0=ALU.mult), [b_cu, b_cw], [b_cy])
                    sc.emit(dve, lambda e, cu=cu, cy=cy, cw=cw: e.scalar_tensor_tensor(
                        out=cy[:], in0=cu[:, 1:S + 1], scalar=cw[:, 1:2], in1=cy[:], op0=ALU.mult, op1=ALU.add),
                        [b_cu, b_cw, b_cy], [b_cy])
                    sc.emit(dve, lambda e, cu=cu, cy=cy, cw=cw: e.scalar_tensor_tensor(
                        out=cy[:], in0=cu[:, 2:S + 2], scalar=cw[:, 2:3], in1=cy[:], op0=ALU.mult, op1=ALU.add),
                        [b_cu, b_cw, b_cy], [b_cy])
                    sc.emit(dve, lambda e, cy=cy, cyo=cyo, cvb=cvb: e.tensor_tensor(out=cyo[:], in0=cy[:], in1=cvb[0][:],
                                                                                    op=ALU.mult), [b_cy, b_cvb[0]], [b_cyo])
                    dma_pool(yT[1536 + ch * 128:1536 + (ch + 1) * 128, :], cyo[:], [b_cyo], [b_yT])
            sc.barrier()

            with ExitStack() as es:
                b_ac = Buf("attconst")
                cs = {}
                for k_, shape, dt in [("c_mstrict", [128, 128], BF16), ("c_mincl", [128, 128], BF16),
                                      ("c_mpair", [128, 256], BF16), ("c_negu", [128, 128], BF16),
                                      ("c_ind", [128, 160], BF16), ("c_negep", [128, 4096], BF16),
                                      ("c_onesp", [128, 256], BF16), ("c_neg8i", [128, 128], BF16),
                                      ("c_ones128", [128, 128], BF16), ("c_pastneg", [128, 512], F32),
                                      ("c_negbig", [128, 512], F32)]:
                    t_ = sb(k_, shape, dt, es)
                    cs[k_] = t_
                    dma_pool(t_[:], cin[k_][:], [], [b_ac])
                cv_open(es, 3)
                n_iter_pts = 3 * 4 * 2 * NTG
                att_tgt = sum(1 for t in conv_tasks if t[1] <= l or (t[1] == l + 1 and t[0] in WNAMES[:4]))
                remaining = max(0, att_tgt - cvs["pos"])
                per_pt = 0
                w_tot = 2 * 4 * sum(4 * g_ + 4 for g_ in range(NTG)) * 1.6 + 0.2 * 64
                quota = {"acc": 0.0}

                def wpump(w):
                    if remaining <= 0:
                        return
                    quota["acc"] += 1.15 * remaining * w / w_tot
                    k_ = int(quota["acc"])
                    if k_ > 0:
                        quota["acc"] -= k_
                        pump(k_, [pool])
                qzs = [[sb("qz%d_%d" % (i, hh), [128, S], BF16, es) for hh in range(2)] for i in range(2)]
                kTs = [sb("kT%d" % i, [128, S], BF16, es) for i in range(2)]
                vzs = [[sb("vz%d_%d" % (i, hh), [128, NQT, 128], BF16, es) for hh in range(2)] for i in range(2)]
                b_qs, b_ks, b_vs = [Buf(), Buf()], [Buf(), Buf()], [Buf(), Buf()]
                for i in range(2):
                    for hh in range(2):
                        o0 = 64 * (1 - hh)
                        sc.emit(pool, lambda e, t=qzs[i][hh], o0=o0: e.memset(t[o0:o0 + 64, :], 0.0), [], [b_qs[i]])
                        sc.emit(pool, lambda e, t=vzs[i][hh], o0=o0: e.memset(t[:, :, o0:o0 + 64], 0.0), [], [b_vs[i]])
                qaugs = [sb("qaug%d" % i, [128, S], BF16, es) for i in range(2)]
                kaugs = [sb("kaug%d" % i, [128, S], BF16, es) for i in range(2)]
                b_qaugs, b_kaugs = [Buf(), Buf()], [Buf(), Buf()]
                for i in range(2):
                    sc.emit(pool, lambda e, t=qaugs[i]: e.memset(t[:], 0.0), [], [b_qaugs[i]])
                    sc.emit(pool, lambda e, t=kaugs[i]: e.memset(t[:], 0.0), [], [b_kaugs[i]])
                NSP = 4
                sp_sb = [sb("sp_sb%d" % i, [128, 512], BF16, es) for i in range(NSP)]
                b_sps = [Buf() for _ in range(NSP)]
                carry_bf = [sb("carry%d" % i, [128, 512], BF16, es) for i in range(2)]
                b_carry = [Buf() for _ in range(2)]
                zero_bf = sb("zero_bf", [128, 512], BF16, es)
                sc.emit(pool, lambda e: e.memset(zero_bf[:], 0.0), [], [b_ac])
                e_sb = [sb("e_sb%d" % i, [128, 512], F32, es) for i in range(2)]
                b_e = [Buf() for _ in range(2)]
                NA = 4
                a_sb = [sb("a_sb%d" % i, [128, 512], BF16, es) for i in range(NA)]
                b_a = [Buf() for _ in range(NA)]
                tot_sb = sb("tot_sb", [128, 512], BF16, es)
                b_tot = Buf("tot")
                ost = [sb("ost%d" % i, [128, 512], BF16, es) for i in range(2)]
                b_ost = [Buf() for _ in range(2)]
                rden = sb("rden", [128, 512], F32, es)
                b_rden = Buf("rden")
                esink = sb("esink", [128, 2], F32, es)
                b_esink = Buf("esink")
                kmeans = [sb("kmean%d" % i, [128, 16], F32, es) for i in range(2)]
                kmTs = [sb("kmT%d" % i, [128, 16], BF16, es) for i in range(2)]
                b_kms = [Buf(), Buf()]
                gm = sb("gm", [128, 32, 16], F32, es)
                top8 = sb("top8", [128, 32, 8], F32, es)
                selb = sb("selb", [128, 32, 16], BF16, es)
                lt = sb("lt", [128, 32, 16], F32, es)
                b_gm, b_top8, b_selb, b_lt = Buf("gm"), Buf("top8"), Buf("selb"), Buf("lt")
                ctr = {"z": 0, "g": 0, "e": 0, "a": 0, "o": 0, "ost": 0, "sp": 0, "mz": 0}

                def pipeline(items, stage_c, stage_d, la=2, hooks=None):
                    n_ = len(items)
                    for i in range(n_ + la):
                        if hooks and i in hooks:
                            hooks[i]()
                        if i < n_:
                            stage_c(items[i])
                        if i - la >= 0:
                            stage_d(items[i - la])

                def qk_cols(g, ks):
                    col0 = max(0, ks - 4 * g) * 128
                    return col0, 512 - col0

                pair_jobs = [("sb", p) for p in range(4)] + [("moba", p) for p in range(4)] + [("swa", p) for p in range(4)]

                def load_pair(ji):
                    kind, p = pair_jobs[ji]
                    i = ji % 2
                    if kind == "sb":
                        qrow, krow, vcol = R_QB + p * 128, R_KB + p * 128, 512 + p * 128
                    elif kind == "moba":
                        qrow, krow, vcol = R_QA + p * 128, R_KA + p * 128, p * 128
                    else:
                        qrow, vcol = R_QC + p * 128, 1024 + (p // 2) * 64
                    for hh in range(2):
                        dma_pool(qzs[i][hh][64 * hh:64 * hh + 64, :], featT[qrow + 64 * hh:qrow + 64 * hh + 64, :],
                                 [b_featT], [b_qs[i]])
                    if kind == "swa":
                        kr = R_KC + (p // 2) * 64
                        dma_pool(kTs[i][0:64, :], featT[kr:kr + 64, :], [b_featT], [b_ks[i]])
                        dma_pool(kTs[i][64:128, :], featT[kr:kr + 64, :], [b_featT], [b_ks[i]])
                    else:
                        dma_pool(kTs[i][:, :], featT[krow:krow + 128, :], [b_featT], [b_ks[i]])
                    for hh in range(2):
                        vc = vcol if kind == "swa" else vcol + 64 * hh
                        src_ap = vscr[:, vc:vc + 64].rearrange("(t p) c -> p t c", p=128)
                        dma_pool(vzs[i][hh][:, :, 64 * hh:64 * hh + 64], src_ap, [b_vscr], [b_vs[i]])

                def evac_norm_store(nb_, db_, base, row, g, add_sink):
                    if add_sink:
                        sc.emit(act, lambda e: e.activation(
                            out=rden[base:base + 64, :], in_=banks[db_][base:base + 64, :], func=AF.Ln,
                            bias=esink[base:base + 64, 1:2], scale=1.0), [b_bank[db_], b_esink], [b_rden])
                        sc.emit(act, lambda e: e.activation(
                            out=rden[base:base + 64, :], in_=rden[base:base + 64, :], func=AF.Exp, scale=-1.0),
                            [b_rden], [b_rden])
                    else:
                        sc.emit(dve, lambda e: e.reciprocal(out=rden[base:base + 64, :], in_=banks[db_][base:base + 64, :]),
                                [b_bank[db_]], [b_rden])
                    si = ctr["ost"] % 2
                    ctr["ost"] += 1
                    sc.emit(dve, lambda e: e.tensor_tensor(
                        out=ost[si][base:base + 64, :], in0=banks[nb_][base:base + 64, :],
                        in1=rden[base:base + 64, :], op=ALU.mult), [b_bank[nb_], b_rden], [b_ost[si]])
                    dma_pool(yT[row + base:row + base + 64, g * 512:(g + 1) * 512],
                             ost[si][base:base + 64, :], [b_ost[si]], [b_yT])

                load_pair(0)
                for ji, (kind, p) in enumerate(pair_jobs):
                    if ji + 1 < len(pair_jobs):
                        load_pair(ji + 1)
                    pi = ji % 2
                    kT = kTs[pi]
                    b_q, b_k, b_v = b_qs[pi], b_ks[pi], b_vs[pi]

                    if kind == "sb":
                        for g in range(NTG):
                            kmax = 4 * g + 3
                            ob, tb = 5, 4
                            for hh in range(2):
                                base = 64 * hh
                                qz, vz = qzs[pi][hh], vzs[pi][hh]
                                info = {}
                                mm(banks[tb][:], cs["c_ones128"][:], zero_bf[:], True, False, [b_ac], [b_bank[tb]])
                                mm(banks[ob][:], cs["c_ones128"][:], zero_bf[:], True, False, [b_ac], [b_bank[ob]])

                                def s1(ks, qz=qz, kT=kT, g=g, info=info):
                                    col0, n = qk_cols(g, ks)
                                    diag = ks >= 4 * g
                                    bk = ctr["z"] % 4
                                    ctr["z"] += 1
                                    mm(banks[bk][:, col0:512], kT[:, ks * 128:(ks + 1) * 128],
                                       qz[:, g * 512 + col0:(g + 1) * 512], True, False, [b_k, b_q], [b_bank[bk]])
                                    if diag:
                                        mm(banks[bk][:, col0:col0 + 128], ident[:], cs["c_mstrict"][:], False, False,
                                           [b_const, b_ac], [b_bank[bk]])
                                    ei = ctr["e"] % 2
                                    ctr["e"] += 1
                                    si_ = ctr["sp"] % NSP
                                    ctr["sp"] += 1
                                    info[ks] = (bk, col0, si_)
                                    sc.emit(act, lambda e, bk=bk, ei=ei, col0=col0: e.activation(
                                        out=e_sb[ei][:, col0:512], in_=banks[bk][:, col0:512], func=AF.Exp, scale=0.125),
                                        [b_bank[bk]], [b_e[ei]])
                                    sc.emit(act, lambda e, ei=ei, si_=si_, col0=col0: e.activation(
                                        out=sp_sb[si_][:, col0:512], in_=e_sb[ei][:, col0:512], func=AF.Ln,
                                        scale=1.0, bias=1.0), [b_e[ei]], [b_sps[si_]])

                                def s2(ks, kmax=kmax, info=info, tb=tb):
                                    bk, col0, si_ = info[ks]
                                    first = ks == kmax
                                    mm(banks[bk][:, col0:512], cs["c_negu"][:], sp_sb[si_][:, col0:512], False, first,
                                       [b_sps[si_], b_ac], [b_bank[bk]])
                                    if not first:
                                        ci_ = (ks + 1) % 2
                                        mm(banks[bk][:, col0:512], cs["c_neg8i"][:], carry_bf[ci_][:, col0:512], False, True,
                                           [b_carry[ci_], b_ac], [b_bank[bk]])
                                    if ks > 0:
                                        mm(banks[tb][:, col0:512], cs["c_ones128"][:], sp_sb[si_][:, col0:512], False, False,
                                           [b_sps[si_], b_ac], [b_bank[tb]])
                                        co_ = ks % 2
                                        sc.emit(dve, lambda e, co_=co_, tb=tb: e.tensor_copy(out=carry_bf[co_][:], in_=banks[tb][:]),
                                                [b_bank[tb]], [b_carry[co_]])
                                    ai = ctr["a"] % NA
                                    ctr["a"] += 1
                                    info[ks] = (bk, col0, si_, ai)
                                    sc.emit(act, lambda e, bk=bk, ai=ai, col0=col0: e.activation(
                                        out=a_sb[ai][:, col0:512], in_=banks[bk][:, col0:512], func=AF.Exp, scale=0.125),
                                        [b_bank[bk]], [b_a[ai]])

                                def s3(ks, info=info, vz=vz, ob=ob):
                                    bk, col0, si_, ai = info[ks]
                                    mm(banks[ob][:, col0:512], vz[:, ks, :], a_sb[ai][:, col0:512], False, ks == 0,
                                       [b_v, b_a[ai]], [b_bank[ob]])

                                order = list(range(kmax, -1, -1))
                                n_ = len(order)
                                for i in range(n_ + 2):
                                    if i < n_:
                                        s1(order[i])
                                    if 0 <= i - 1 < n_:
                                        s2(order[i - 1])
                                    if 0 <= i - 2 < n_:
                                        s3(order[i - 2])
                                si = ctr["ost"] % 2
                                ctr["ost"] += 1
                                sc.emit(dve, lambda e, si=si, ob=ob, base=base: e.tensor_copy(
                                    out=ost[si][base:base + 64, :], in_=banks[ob][base:base + 64, :]),
                                    [b_bank[ob]], [b_ost[si]])
                                r_ = 512 + p * 128 + base
                                dma_pool(yT[r_:r_ + 64, g * 512:(g + 1) * 512], ost[si][base:base + 64, :],
                                         [b_ost[si]], [b_yT])
                                wpump(4 * g + 4)

                    elif kind == "moba":
                        nq16 = NQT * 16
                        GB = 3
                        moba_heads = [(ji2, p2, hh2) for ji2, (k2, p2) in enumerate(pair_jobs) if k2 == "moba" for hh2 in range(2)]

                        def prep_a(m):
                            ji2, p2, hh2 = moba_heads[m]
                            pi2 = ji2 % 2
                            kT2, qz2 = kTs[pi2], qzs[pi2][hh2]
                            kmean, kmT, b_km = kmeans[p2 % 2], kmTs[p2 % 2], b_kms[p2 % 2]
                            if hh2 == 0:
                                sc.emit(dve, lambda e, kT2=kT2, kmean=kmean: e.tensor_reduce(
                                    out=kmean[:, 0:NBLK], in_=kT2[:, :].rearrange("p (n j) -> p n j", j=256),
                                    axis=AX.X, op=ALU.add), [b_ks[pi2]], [b_km])
                                if NBLK < 16:
                                    sc.emit(dve, lambda e, kmT=kmT: e.memset(kmT[:, NBLK:16], 0.0), [], [b_km])
                                sc.emit(dve, lambda e, kmT=kmT, kmean=kmean: e.tensor_scalar(
                                    out=kmT[:, 0:NBLK], in0=kmean[:, 0:NBLK], scalar1=1.0 / 256.0,
                                    scalar2=None, op0=ALU.mult), [b_km], [b_km])
                            hd2 = 2 * p2 + hh2
                            qaug, kaug, b_qaug, b_kaug = qaugs[hh2], kaugs[hh2], b_qaugs[hh2], b_kaugs[hh2]
                            dma_pool(qaug[16:20, :], cin["c_qaug_a"][hd2], [], [b_qaug])
                            dma_pool(kaug[0:20, :], cin["c_kaug_a"][hd2], [], [b_kaug])
                            for tq in range(NQT):
                                mm(banks[GB][:, tq * 16:(tq + 1) * 16], qz2[:, tq * 128:(tq + 1) * 128],
                                   kmT[:, :], True, True, [b_qs[pi2], b_km], [b_bank[GB]])
                            gflat = gm[:].rearrange("p a b -> p (a b)")
                            sc.emit(dve, lambda e: e.tensor_tensor(out=gflat[:, 0:nq16], in0=banks[GB][:, 0:nq16],
                                                                   in1=cs["c_pastneg"][:, 0:nq16], op=ALU.add),
                                    [b_bank[GB], b_ac], [b_gm])
                            for tq in range(NQT):
                                sc.emit(dve, lambda e, tq=tq: e.max(out=top8[:, tq, :], in_=gm[:, tq, :]), [b_gm], [b_top8])
                            sc.emit(dve, lambda e: e.tensor_tensor(
                                out=lt[:, 0:NQT, :], in0=gm[:, 0:NQT, :],
                                in1=top8[:, 0:NQT, 2:3].broadcast_to([128, NQT, 16]), op=ALU.is_lt),
                                [b_gm, b_top8], [b_lt])
                            sc.emit(dve, lambda e: e.tensor_tensor(
                                out=selb[:, 0:NQT, :], in0=lt[:, 0:NQT, :],
                                in1=cs["c_negbig"][:, 0:nq16].rearrange("p (a b) -> p a b", b=16), op=ALU.mult),
                                [b_lt, b_ac], [b_selb])

                        def prep_b(m):
                            ji2, p2, hh2 = moba_heads[m]
                            qaug, b_qaug = qaugs[hh2], b_qaugs[hh2]
                            for t8 in range(0, NQT, 8):
                                pv = banks[GB][:].bitcast(BF16)
                                for jj in range(8):
                                    tq = t8 + jj
                                    sc.emit(pe, lambda e, pv=pv, jj=jj, tq=tq: e.transpose(
                                        pv[0:16, jj * 128:(jj + 1) * 128], selb[:, tq, :], ident[:]),
                                        [b_selb, b_const], [b_bank[GB]])
                                sc.emit(act, lambda e, pv=pv, t8=t8, qaug=qaug: e.copy(
                                    out=qaug[0:16, t8 * 128:(t8 + 8) * 128], in_=pv[0:16, 0:1024]),
                                    [b_bank[GB]], [b_qaug])

                        for hh in range(2):
                            m = moba_heads.index((ji, p, hh))
                            if m == 0:
                                prep_a(0)
                                prep_b(0)
                            base = 64 * hh
                            qz, vz = qzs[pi][hh], vzs[pi][hh]
                            qaug, kaug, b_qaug, b_kaug = qaugs[hh], kaugs[hh], b_qaugs[hh], b_kaugs[hh]
                            nbs = {}

                            def mo_c(item, qz=qz, kT=kT, nbs=nbs, qaug=qaug, kaug=kaug, b_qaug=b_qaug, b_kaug=b_kaug):
                                g, ks = item
                                bk = ctr["mz"] % 3
                                ctr["mz"] += 1
                                col0, n = qk_cols(g, ks)
                                diag = ks >= 4 * g
                                mm(banks[bk][:, col0:512], kT[:, ks * 128:(ks + 1) * 128],
                                   qz[:, g * 512 + col0:(g + 1) * 512], True, False,
                                   [b_k, b_q], [b_bank[bk]])
                                mm(banks[bk][:, col0:512], kaug[:, ks * 128:(ks + 1) * 128],
                                   qaug[:, g * 512 + col0:(g + 1) * 512], False, not diag,
                                   [b_kaug, b_qaug], [b_bank[bk]])
                                if diag:
                                    mm(banks[bk][:, col0:col0 + 128], ident[:], cs["c_mincl"][:], False, True,
                                       [b_const, b_ac], [b_bank[bk]])
                                ai = ctr["a"] % NA
                                ctr["a"] += 1
                                nbs[item] = ai
                                sc.emit(act, lambda e, bk=bk, ai=ai, col0=col0: e.activation(
                                    out=a_sb[ai][:, col0:512], in_=banks[bk][:, col0:512], func=AF.Exp, scale=0.125),
                                    [b_bank[bk]], [b_a[ai]])

                            def mo_d(item, base=base, p=p, vz=vz, hh=hh, nbs=nbs):
                                g, ks = item
                                kmax = 4 * g + 3
                                col0, n = qk_cols(g, ks)
                                ai = nbs[item]
                                nb_ = 4 + (g % 2)
                                db_ = 6 + (g % 2)
                                mm(banks[nb_][:, col0:512], vz[:, ks, :], a_sb[ai][:, col0:512],
                                   ks == 0, ks == kmax, [b_v, b_a[ai]], [b_bank[nb_]])
                                mm(banks[db_][:, col0:512], cs["c_onesp"][:, hh * 128:(hh + 1) * 128], a_sb[ai][:, col0:512],
                                   ks == 0, ks == kmax, [b_ac, b_a[ai]], [b_bank[db_]])
                                if ks == kmax:
                                    evac_norm_store(nb_, db_, base, p * 128, g, False)
                                    wpump(0.6 * (4 * g + 4))

                            items = [(g, ks) for g in range(NTG) for ks in range(4 * g + 4)]
                            hooks = None
                            if m + 1 < len(moba_heads):
                                hooks = {len(items) // 2: (lambda m=m: prep_a(m + 1)),
                                         (3 * len(items)) // 4: (lambda m=m: prep_b(m + 1))}
                            pipeline(items, mo_c, mo_d, la=2, hooks=hooks)

                    else:
                        if p == 0:
                            for i_ in range(2):
                                sc.emit(pool, lambda e, t=qaugs[i_]: e.memset(t[0:16, :], 0.0), [], [b_qaugs[i_]])
                        dma_pool(esink[0:64, 0:1], sinks[l, 2 * p:2 * p + 1].partition_broadcast(64), [], [b_esink])
                        dma_pool(esink[64:128, 0:1], sinks[l, 2 * p + 1:2 * p + 2].partition_broadcast(64), [], [b_esink])
                        sc.emit(act, lambda e: e.activation(out=esink[:, 1:2], in_=esink[:, 0:1], func=AF.Exp),
                                [b_esink], [b_esink])
                        for hh in range(2):
                            hd_ = 2 * p + hh
                            base = 64 * hh
                            qz, vz = qzs[pi][hh], vzs[pi][hh]
                            qaug, kaug, b_qaug, b_kaug = qaugs[hh], kaugs[hh], b_qaugs[hh], b_kaugs[hh]
                            dma_pool(qaug[16:20, :], cin["c_qaug_c"][hd_], [], [b_qaug])
                            dma_pool(kaug[16:20, :], cin["c_kaug_c"][hd_], [], [b_kaug])
                            nbs = {}

                            def sw_c(tq, qz=qz, kT=kT, nbs=nbs, qaug=qaug, kaug=kaug, b_qaug=b_qaug, b_kaug=b_kaug):
                                bk = ctr["z"] % 4
                                ctr["z"] += 1
                                c_lo = 0 if tq > 0 else 128
                                qs = slice(tq * 128, (tq + 1) * 128)
                                ksl = [ks for ks in (tq - 1, tq) if ks >= 0]
                                for ks in ksl:
                                    w_ = ks - (tq - 1)
                                    ws = slice(w_ * 128, (w_ + 1) * 128)
                                    mm(banks[bk][:, ws], kT[:, ks * 128:(ks + 1) * 128],
                                       qz[:, qs], True, False, [b_k, b_q], [b_bank[bk]])
                                    mm(banks[bk][:, ws], kaug[:, ks * 128:(ks + 1) * 128],
                                       qaug[:, qs], False, False, [b_kaug, b_qaug], [b_bank[bk]])
                                    mm(banks[bk][:, ws], ident[:], cs["c_mpair"][:, ws], False, True,
                                       [b_const, b_ac], [b_bank[bk]])
                                ai = ctr["a"] % NA
                                ctr["a"] += 1
                                nbs[tq] = ai
                                sc.emit(act, lambda e, bk=bk, ai=ai, c_lo=c_lo: e.activation(
                                    out=a_sb[ai][:, c_lo:256], in_=banks[bk][:, c_lo:256], func=AF.Exp, scale=0.125),
                                    [b_bank[bk]], [b_a[ai]])

                            def sw_d(tq, base=base, p=p, vz=vz, hh=hh, nbs=nbs):
                                g, t4 = tq // 4, tq % 4
                                ai = nbs[tq]
                                nb_ = 4 + (g % 2)
                                db_ = 6 + (g % 2)
                                ksl = [ks for ks in (tq - 1, tq) if ks >= 0]
                                for idx, ks in enumerate(ksl):
                                    w_ = ks - (tq - 1)
                                    mm(banks[nb_][:, t4 * 128:(t4 + 1) * 128], vz[:, ks, :],
                                       a_sb[ai][:, w_ * 128:(w_ + 1) * 128], idx == 0, idx == len(ksl) - 1,
                                       [b_v, b_a[ai]], [b_bank[nb_]])
                                for idx, ks in enumerate(ksl):
                                    w_ = ks - (tq - 1)
                                    mm(banks[db_][:, t4 * 128:(t4 + 1) * 128], cs["c_onesp"][:, hh * 128:(hh + 1) * 128],
                                       a_sb[ai][:, w_ * 128:(w_ + 1) * 128], idx == 0, idx == len(ksl) - 1,
                                       [b_ac, b_a[ai]], [b_bank[db_]])
                                if t4 == 3:
                                    evac_norm_store(nb_, db_, base, 1024 + p * 128, g, True)
                                    wpump(0.2)

                            pipeline(list(range(NQT)), sw_c, sw_d, la=2)
                if cvs["pos"] < att_tgt:
                    pump(att_tgt - cvs["pos"], [dve, pool])
                cv_flush()
            sc.barrier()

        for l in range(L + 1):
            token_phase(first=(l == 0), last=(l == L), l=l)
            if l < L:
                attention_phase(l)

        sc.emit(sp, lambda e: e.nop(), [b_out], [])

        with nc.Block() as block:
            @block.tensor
            def _(e):
                sc.replay(sc.pe, e)

            @block.scalar
            def _(e):
                sc.replay(sc.act, e)

            @block.vector
            def _(e):
                sc.replay(sc.dve, e)

            @block.gpsimd
            def _(e):
                sc.replay(sc.pool, e)

            @block.sync
            def _(e):
                sc.replay(sc.sp, e)
    return nc, consts


def make_in_maps(inputs, consts, L, nb):
    x = np.ascontiguousarray(inputs["x"], dtype=np.float32)
    shared = {}
    for n in WNAMES:
        w = np.asarray(inputs[n], dtype=np.float32)
        shared[n] = np.ascontiguousarray(w.reshape((L,) + WSHAPES[n]))
    for n in ["ffn1_norm", "mix_norm", "ffn2_norm"]:
        shared[n] = np.ascontiguousarray(inputs[n], dtype=np.float32)
    shared["final_norm"] = np.ascontiguousarray(np.asarray(inputs["final_norm"], np.float32).reshape(1, D))
    shared["conv_w"] = np.ascontiguousarray(np.asarray(inputs["conv_w"], np.float32).reshape(L, 3, 512))
    shared["attn_sinks"] = np.ascontiguousarray(inputs["attn_sinks"], dtype=np.float32)
    shared.update(consts)
    return [dict(shared, x=x[b]) for b in range(nb)]


_CACHE = {}


def kernel(**inputs):
    S, L = 4096, 2
    if "prog" not in _CACHE:
        _CACHE["prog"] = build(S, L)
    nc, consts = _CACHE["prog"]
    B = np.asarray(inputs["x"]).shape[0]
    in_maps = make_in_maps(inputs, consts, L, B)
    res = run_bass_kernel_spmd(nc, in_maps, core_ids=list(range(B)))
    return np.stack([np.asarray(r["out"]) for r in res.results], axis=0).astype(np.float32)
```

```python
import numpy as np
import concourse.bass as bass
import concourse.mybir as mybir
from concourse.bass_utils import run_bass_kernel_spmd

F32 = mybir.dt.float32
BF16 = mybir.dt.bfloat16
AF = mybir.ActivationFunctionType
ALU = mybir.AluOpType
AX = mybir.AxisListType

D = 2048
DFF = 5632
KC = D // 128
FC = DFF // 128
EPS = 1e-6
NEG = -32768.0
O_QA, O_KA, O_VA, O_QB, O_KB, O_VB, O_QC, O_KC, O_VC, O_BG, O_CG, O_HD, O_GATE = (
    0, 512, 1024, 1536, 2048, 2560, 3072, 3584, 3712, 3840, 4352, 4864, 5376)
IN_COLS = 13568
R_QA, R_KA, R_QB, R_KB, R_QC, R_KC, R_BG, R_CG, R_HD = 0, 512, 1024, 1536, 2048, 2560, 2688, 3200, 3712
FEAT_ROWS = 4224
FM_SEGS = [(O_QA, R_QA, 512), (O_KA, R_KA, 512), (O_QB, R_QB, 512), (O_KB, R_KB, 512), (O_QC, R_QC, 512),
           (O_KC, R_KC, 128), (O_BG, R_BG, 512), (O_CG, R_CG, 512), (O_HD, R_HD, 512)]
VW = 1152


class Src:
    def __init__(self, name, k, inc):
        self.name, self.k, self.inc = name, k, inc
        self.count = 0
        self.sems = None


class Stream:
    def __init__(self, name, inorder=False):
        self.name = name
        self.ops = []
        self.seen = {}
        self.inorder = inorder
        self.src = Src(name, 1, 1)


class Buf:
    __slots__ = ("w", "r", "name")

    def __init__(self, name=""):
        self.w = {}
        self.r = {}
        self.name = name


class Sched:
    def __init__(self):
        self.pe = Stream("pe", inorder=True)
        self.act = Stream("act")
        self.dve = Stream("dve")
        self.pool = Stream("pool")
        self.sp = Stream("sp")
        self.streams = [self.pe, self.act, self.dve, self.pool, self.sp]
        self.q_sp = Src("qsp", 12, 16)
        self.q_pool = Src("qpool", 8, 16)
        self.srcs = [s.src for s in self.streams] + [self.q_sp, self.q_pool]

    def _wait(self, st, ev):
        src, slot, val = ev
        if src is st.src and st.inorder:
            return
        key = (id(src), slot)
        if st.seen.get(key, 0) < val:
            st.ops.append(("w", src, slot, val))
            st.seen[key] = val

    def emit(self, st, fn, reads=(), writes=(), q=None):
        deps = []
        for b in reads:
            deps.extend((s, sl, v) for (s, sl), v in b.w.values())
        for b in writes:
            deps.extend((s, sl, v) for (s, sl), v in b.w.values())
            deps.extend((s, sl, v) for (s, sl), v in b.r.values())
        for ev in deps:
            self._wait(st, ev)
        src = q if q is not None else st.src
        if src.k > 1:
            i = src.count
            slot = i % src.k
            val = 16 * (i // src.k + 1)
            if i >= src.k:
                self._wait(st, (src, slot, val - 16))
            src.count += 1
        else:
            src.count += 1
            slot, val = 0, src.count
        st.ops.append(("op", fn, src, slot))
        ev = (src, slot, val)
        for b in reads:
            key = (id(src), slot)
            old = b.r.get(key)
            if old is None or old[1] < val:
                b.r[key] = ((src, slot), val)
        for b in writes:
            b.w[(id(src), slot)] = ((src, slot), val)
            b.r = {}
        return ev

    def barrier(self):
        evs = []
        for src in self.srcs:
            if src.count == 0:
                continue
            if src.k == 1:
                evs.append((src, 0, src.count))
            else:
                for slot in range(min(src.k, src.count)):
                    i_last = ((src.count - 1 - slot) // src.k) * src.k + slot
                    evs.append((src, slot, 16 * (i_last // src.k + 1)))
        for st in self.streams:
            for ev in evs:
                self._wait(st, ev)

    def replay(self, st, eng):
        for op in st.ops:
            if op[0] == "w":
                _, src, slot, val = op
                eng.wait_ge(src.sems[slot], val)
            else:
                _, fn, src, slot = op
                fn(eng).then_inc(src.sems[slot], src.inc)


def _bf(x):
    import ml_dtypes
    return np.asarray(x, np.float32).astype(ml_dtypes.bfloat16).astype(np.float32)


def make_consts(S):
    nqt = S // 128
    j = np.arange(128)[:, None]
    i = np.arange(128)[None, :]
    c = {}
    c["c_ident"] = np.eye(128, dtype=np.float32)
    c["c_mstrict"] = np.where(j < i, 0.0, NEG).astype(np.float32)
    c["c_mincl"] = np.where(j <= i, 0.0, NEG).astype(np.float32)
    c["c_mpair"] = np.concatenate([np.where(j > i, 0.0, NEG), np.where(j <= i, 0.0, NEG)], axis=1).astype(np.float32)
    c["c_negu"] = np.where(j >= i, -8.0, 0.0).astype(np.float32)
    ind = np.zeros((128, 160), np.float32)
    ind[:, 31] = 1.0
    c["c_ind"] = ind
    nep = np.zeros((128, 32, 128), np.float32)
    for ks in range(32):
        nep[ks + 1:32, ks, :] = -8.0
    c["c_negep"] = nep.reshape(128, 4096)
    onesp = np.zeros((128, 256), np.float32)
    onesp[:, 0:64] = 1.0
    onesp[:, 128 + 64:256] = 1.0
    c["c_onesp"] = onesp
    c["c_neg8i"] = (-8.0 * np.eye(128)).astype(np.float32)
    c["c_ones128"] = np.ones((128, 128), np.float32)
    tq = np.arange(32)[:, None]
    n = np.arange(16)[None, :]
    past = (n < (tq // 2))
    c["c_pastneg"] = np.broadcast_to(np.where(past, 0.0, -1e30).reshape(1, 512), (128, 512)).astype(np.float32).copy()
    c["c_negbig"] = np.broadcast_to(np.where(past, NEG, 0.0).reshape(1, 512), (128, 512)).astype(np.float32).copy()
    slopes = 2.0 ** (-8.0 * np.arange(1, 17, dtype=np.float64) / 16.0)
    t = np.arange(S)
    ti, tt = (t % 128).astype(np.float32), ((t // 128) * 128).astype(np.float32)
    blk = t // 256

    def aug(sl, with_sel):
        s8 = 8.0 * float(_bf(sl))
        q = np.stack([ti, tt, np.full(S, s8, np.float32), np.full(S, s8, np.float32)]).astype(np.float32)
        k = np.stack([np.full(S, -s8, np.float32), np.full(S, -s8, np.float32), ti, tt]).astype(np.float32)
        if with_sel:
            sel = (blk[None, :] == np.arange(16)[:, None]).astype(np.float32)
            k = np.concatenate([sel, k], axis=0)
        return q, k

    qa, ka, qc, kc = [], [], [], []
    for h in range(8):
        q, k = aug(slopes[8 + h], True)
        qa.append(q); ka.append(k)
        q, k = aug(slopes[h], False)
        qc.append(q); kc.append(k)
    c["c_qaug_a"] = np.stack(qa)
    c["c_kaug_a"] = np.stack(ka)
    c["c_qaug_c"] = np.stack(qc)
    c["c_kaug_c"] = np.stack(kc)
    return c


WNAMES = ["ffn1_w1", "ffn1_w3", "ffn1_w2", "w_in", "w_branch", "w_out", "ffn2_w1", "ffn2_w3", "ffn2_w2"]
WSHAPES = {"ffn1_w1": (D, DFF), "ffn1_w3": (D, DFF), "ffn1_w2": (DFF, D), "w_in": (D, IN_COLS),
           "w_branch": (4 * 512, D), "w_out": (D, D), "ffn2_w1": (D, DFF), "ffn2_w3": (D, DFF), "ffn2_w2": (DFF, D)}


NORM_IDX = {"ffn1_norm": 0, "mix_norm": 1, "ffn2_norm": 2}


def build(S=4096, L=2, debug=False):
    from contextlib import ExitStack
    assert S % 512 == 0
    NTG, NQT, NBLK = S // 512, S // 128, S // 256
    nc = bass.Bass("TRN2", target_bir_lowering=False)
    sc = Sched()
    consts = make_consts(S)

    def din(name, shape, dt=F32):
        return nc.dram_tensor(name, list(shape), dt, kind="ExternalInput").ap()

    x_in = din("x", [S, D])
    out_d = nc.dram_tensor("out", [S, D], F32, kind="ExternalOutput").ap()
    wsrc = {n: din(n, (L,) + WSHAPES[n]) for n in WNAMES}
    norms = {n: din(n, [L, D]) for n in ["ffn1_norm", "mix_norm", "ffn2_norm"]}
    fnorm = din("final_norm", [1, D])
    conv_w = din("conv_w", [L, 3, 512])
    sinks = din("attn_sinks", [L, 8])
    cin = {k: din(k, v.shape) for k, v in consts.items()}
    skind = "ExternalOutput" if debug else "Internal"
    wbf = {n: nc.dram_tensor("s_" + n, [L] + list(WSHAPES[n]), BF16, kind="Internal").ap() for n in WNAMES}
    hbuf = nc.dram_tensor("s_h", [S, D], F32, kind=skind).ap()
    featT = nc.dram_tensor("s_featT", [FEAT_ROWS, S], BF16, kind=skind).ap()
    vscr = nc.dram_tensor("s_v", [S, VW], BF16, kind=skind).ap()
    yT = nc.dram_tensor("s_yT", [D, S], BF16, kind=skind).ap()
    b_hbuf, b_featT, b_vscr, b_yT = Buf("hbuf"), Buf("featT"), Buf("vscr"), Buf("yT")
    if debug:
        d_gm = nc.dram_tensor("d_gm", [128, 512], F32, kind="ExternalOutput").ap()
        d_top8 = nc.dram_tensor("d_top8", [128, 256], F32, kind="ExternalOutput").ap()
        d_lt = nc.dram_tensor("d_lt", [128, 512], F32, kind="ExternalOutput").ap()
        d_km = nc.dram_tensor("d_km", [128, 16], F32, kind="ExternalOutput").ap()
        b_dbg = Buf("dbg")
    b_w = {n: [Buf(n + str(l)) for l in range(L)] for n in WNAMES}
    b_out = Buf("out")

    with ExitStack() as top:
        uid = [0]

        def sb(name, shape, dt, es=top):
            uid[0] += 1
            return es.enter_context(nc.sbuf_tensor("t%d_%s" % (uid[0], name), list(shape), dt))

        banks = [top.enter_context(nc.psum_tensor("bank%d" % i, [128, 512], F32)) for i in range(8)]
        b_bank = [Buf("bank%d" % i) for i in range(8)]
        for s_ in sc.srcs:
            s_.sems = [top.enter_context(nc.semaphore("%s_%d" % (s_.name, i))) for i in range(s_.k)]

        pe, act, dve, pool, sp = sc.pe, sc.act, sc.dve, sc.pool, sc.sp

        def dma_sp(out, in_, reads, writes):
            sc.emit(sp, lambda e: e.dma_start(out=out, in_=in_), reads, writes, q=sc.q_sp)

        def dma_pool(out, in_, reads, writes):
            sc.emit(pool, lambda e: e.dma_start(out=out, in_=in_), reads, writes, q=sc.q_pool)

        def mm(out, lhsT, rhs, start, stop, reads, writes):
            sc.emit(pe, lambda e: e.matmul(out, lhsT, rhs, start=start, stop=stop), reads, writes)

        b_const = Buf("const")
        ident = sb("c_ident", [128, 128], BF16)
        dma_pool(ident[:], cin["c_ident"][:], [], [b_const])
        gT = sb("gT", [128, 3 * L, KC], F32)
        for l in range(L):
            for n, ni in NORM_IDX.items():
                for k in range(KC):
                    dma_pool(gT[:, l * 3 + ni, k:k + 1],
                             norms[n][l, k * 128:(k + 1) * 128].rearrange("(c o) -> c o", o=1), [], [b_const])

        conv_tasks = []
        for l in range(L):
            for n in WNAMES:
                K_, N_ = WSHAPES[n]
                nch = -(-N_ // 1408)
                while N_ % nch:
                    nch += 1
                cw_ = N_ // nch
                for r0 in range(0, K_, 128):
                    for ci in range(nch):
                        conv_tasks.append((n, l, r0, ci * cw_, cw_))
        cvs = {"pos": 0, "pend": [], "bufs": None, "it": 0}

        def cv_open(es, nb=4):
            fin = [sb("cv_in%d" % i, [128, 1408], F32, es) for i in range(nb)]
            fout = [sb("cv_out%d" % i, [128, 1408], BF16, es) for i in range(nb)]
            cvs["bufs"] = (fin, fout, [Buf() for _ in range(nb)], [Buf() for _ in range(nb)], nb)

        def cv_flush():
            for (dst_ap, o_, bfo, bw_) in cvs["pend"]:
                dma_sp(dst_ap, o_, [bfo], [bw_])
            cvs["pend"] = []

        def pump(k, engines):
            fin, fout, b_fin, b_fout, nb = cvs["bufs"]
            for _ in range(k):
                if cvs["pos"] >= len(conv_tasks):
                    break
                n, l, r0, c0, cw = conv_tasks[cvs["pos"]]
                cvs["pos"] += 1
                i = cvs["it"] % nb
                cvs["it"] += 1
                src_ap = wsrc[n][l, r0:r0 + 128, c0:c0 + cw]
                dst_ap = wbf[n][l, r0:r0 + 128, c0:c0 + cw]
                dma_sp(fin[i][:, :cw], src_ap, [], [b_fin[i]])
                o_, a_ = fout[i][:, :cw], fin[i][:, :cw]
                eng = engines[cvs["it"] % len(engines)]
                if eng is act:
                    sc.emit(act, lambda e, o=o_, a=a_: e.copy(out=o, in_=a), [b_fin[i]], [b_fout[i]])
                else:
                    sc.emit(eng, lambda e, o=o_, a=a_: e.tensor_copy(out=o, in_=a), [b_fin[i]], [b_fout[i]])
                cvs["pend"].append((dst_ap, o_, b_fout[i], b_w[n][l]))
                while len(cvs["pend"]) > 2:
                    dst2, o2, bfo2, bw2 = cvs["pend"].pop(0)
                    dma_sp(dst2, o2, [bfo2], [bw2])

        def pump_until(names_layers, engines):
            need = set(names_layers)
            last = -1
            for idx, t in enumerate(conv_tasks):
                if (t[0], t[1]) in need:
                    last = idx
            if last >= cvs["pos"]:
                pump(last + 1 - cvs["pos"], engines)
            cv_flush()

        with ExitStack() as es:
            cv_open(es, 6)
            pump_until([("ffn1_w1", 0), ("ffn1_w3", 0), ("ffn1_w2", 0), ("w_in", 0)], [dve, act, pool])
        sc.barrier()

        def token_phase(first, last, l):
            with ExitStack() as es:
                h = sb("h", [128, 4, D], F32, es)
                b_h = [Buf("h%d" % s) for s in range(4)]
                xnT = sb("xnT", [128, KC, 512], BF16, es)
                b_xnT = Buf("xnT")
                hidT = sb("hidT", [128, FC, 512], BF16, es)
                b_hidT = Buf("hidT")
                NSL = 3
                slots = [sb("wslot%d" % i, [128, 16 * 512], BF16, es) for i in range(NSL)]
                b_slot = [Buf("slot%d" % i) for i in range(NSL)]
                xtok = sb("xtok", [128, D], BF16, es)
                b_xtok = Buf("xtok")
                ss = sb("ss", [128, 8], F32, es)
                b_ss = Buf("ss")
                sgt = [sb("sg%d" % i, [128, 512], F32, es) for i in range(2)]
                b_sg = [Buf() for _ in range(2)]
                if not first:
                    acc = sb("acc", [128, 4, 512], F32, es)
                    b_acc = [Buf() for _ in range(4)]
                    tmp = sb("tmpf", [128, 512], F32, es)
                    b_tmp = Buf("tmp")
                if not last:
                    stg = [sb("stg%d" % i, [128, 4, 512], BF16, es) for i in range(2)]
                    b_stg = [Buf() for _ in range(2)]
                    vst = [sb("vst%d" % i, [128, 512], BF16, es) for i in range(2)]
                    b_vst = [Buf() for _ in range(2)]
                else:
                    gbc = sb("gbc", [128, D], F32, es)
                    b_gbc = Buf("gbc")
                st = {"slot": 0, "bank": 0, "sg": 0, "stg": 0, "vst": 0}
                if last:
                    tgt = len(conv_tasks)
                elif first:
                    tgt = sum(1 for t in conv_tasks if t[1] == 0)
                else:
                    tgt = len(conv_tasks)
                todo = max(0, tgt - cvs["pos"])
                n_pts = NTG * (30 if first else 60)
                tp_per = -(-todo // max(1, n_pts - 20)) if todo > 0 else 0
                if todo > 0:
                    cv_open(es, 3)

                n_win0 = sum(1 for t in conv_tasks if t[1] == 0 and t[0] in WNAMES[:4])
                cur = {"tg": 0}

                def tp_pump():
                    if tp_per > 0 and cvs["pos"] < tgt:
                        k_ = tp_per
                        if first and cur["tg"] == 0 and cvs["pos"] < n_win0:
                            k_ = 12
                        pump(min(k_, tgt - cvs["pos"]), [pool, dve] if (first and cur["tg"] == 0) else [pool])

                def load_panel(W, bW, k0, kn, c0, C):
                    i = st["slot"] % NSL
                    st["slot"] += 1
                    view = slots[i][:, 0:kn * C].rearrange("p (k c) -> p k c", c=C)
                    src_ap = W[k0 * 128:(k0 + kn) * 128, c0:c0 + C].rearrange("(k p) c -> p k c", p=128)
                    dma_sp(view, src_ap, [bW], [b_slot[i]])
                    return view, b_slot[i]

                def rstd_for(s):
                    sc.emit(act, lambda e, s=s: e.activation(out=xtok[:], in_=h[:, s, :], func=AF.Square,
                                                             accum_out=ss[:, 0:1]), [b_h[s]], [b_xtok, b_ss])
                    sc.emit(act, lambda e: e.activation(out=ss[:, 1:2], in_=ss[:, 0:1], func=AF.Sqrt,
                                                        scale=1.0 / D, bias=ss[:, 4:5]), [b_ss], [b_ss])
                    sc.emit(dve, lambda e: e.reciprocal(out=ss[:, 2:3], in_=ss[:, 1:2]), [b_ss], [b_ss])

                sc.emit(dve, lambda e: e.memset(ss[:, 4:5], EPS), [], [b_ss])

                def norm_transpose(gi):
                    for s in range(4):
                        rstd_for(s)
                        sc.emit(dve, lambda e, s=s: e.tensor_scalar(
                            out=xtok[:], in0=h[:, s, :], scalar1=ss[:, 2:3], scalar2=None, op0=ALU.mult),
                            [b_h[s], b_ss], [b_xtok])
                        for j4 in range(4):
                            bk = 6 + (j4 % 2)
                            pv = banks[bk][:].bitcast(BF16)
                            for jj in range(4):
                                kc_ = j4 * 4 + jj
                                sc.emit(pe, lambda e, pv=pv, jj=jj, kc_=kc_: e.transpose(
                                    pv[:, jj * 128:(jj + 1) * 128], xtok[:, kc_ * 128:(kc_ + 1) * 128], ident[:]),
                                    [b_xtok, b_const], [b_bank[bk]])
                            for jj in range(4):
                                kc_ = j4 * 4 + jj
                                if jj % 2 == 0:
                                    sc.emit(act, lambda e, pv=pv, jj=jj, kc_=kc_, s=s: e.activation(
                                        out=xnT[:, kc_, s * 128:(s + 1) * 128], in_=pv[:, jj * 128:(jj + 1) * 128],
                                        func=AF.Copy, scale=gT[:, gi, kc_:kc_ + 1]),
                                        [b_bank[bk], b_const], [b_xnT])
                                else:
                                    sc.emit(dve, lambda e, pv=pv, jj=jj, kc_=kc_, s=s: e.tensor_scalar(
                                        out=xnT[:, kc_, s * 128:(s + 1) * 128], in0=pv[:, jj * 128:(jj + 1) * 128],
                                        scalar1=gT[:, gi, kc_:kc_ + 1], scalar2=None, op0=ALU.mult),
                                        [b_bank[bk], b_const], [b_xnT])

                def mm_tm(xT, bxT, kct, W, bW, ncols, scale, k_off=0):
                    kp = []
                    k0 = 0
                    while k0 < kct:
                        kn = min(16, kct - k0)
                        kp.append((k0, kn))
                        k0 += kn
                    for ci, c0 in enumerate(range(0, ncols, 512)):
                        tp_pump()
                        for (k0, kn) in kp:
                            pw, bpw = load_panel(W, bW, k0, kn, c0, 512)
                            for s in range(4):
                                bk = (ci % 2) * 4 + s
                                for k in range(kn):
                                    kk = k0 + k
                                    mm(banks[bk][:], xT[:, k_off + kk, s * 128:(s + 1) * 128], pw[:, k, :],
                                       kk == 0, kk == kct - 1, [bpw, bxT], [b_bank[bk]])
                        for s in range(4):
                            bk = (ci % 2) * 4 + s
                            sc.emit(dve, lambda e, s=s, c0=c0, bk=bk: e.scalar_tensor_tensor(
                                out=h[:, s, c0:c0 + 512], in0=banks[bk][:], scalar=float(scale),
                                in1=h[:, s, c0:c0 + 512], op0=ALU.mult, op1=ALU.add),
                                [b_bank[bk], b_h[s]], [b_h[s]])

                def ffn(l_, pre):
                    w1, bw1 = wbf[pre + "_w1"][l_], b_w[pre + "_w1"][l_]
                    w3, bw3 = wbf[pre + "_w3"][l_], b_w[pre + "_w3"][l_]
                    w2, bw2 = wbf[pre + "_w2"][l_], b_w[pre + "_w2"][l_]
                    for c0 in range(0, DFF, 512):
                        tp_pump()
                        p1, bp1 = load_panel(w1, bw1, 0, KC, c0, 512)
                        p3, bp3 = load_panel(w3, bw3, 0, KC, c0, 512)
                        for f in range(4):
                            fc = c0 // 128 + f
                            ba = st["bank"] % 2
                            st["bank"] += 1
                            b1, b3 = ba, 2 + ba
                            for k in range(KC):
                                mm(banks[b1][:], p1[:, k, f * 128:(f + 1) * 128], xnT[:, k, :], k == 0, k == KC - 1,
                                   [bp1, b_xnT], [b_bank[b1]])
                            for k in range(KC):
                                mm(banks[b3][:], p3[:, k, f * 128:(f + 1) * 128], xnT[:, k, :], k == 0, k == KC - 1,
                                   [bp3, b_xnT], [b_bank[b3]])
                            si = st["sg"] % 2
                            st["sg"] += 1
                            sc.emit(act, lambda e, si=si, b1=b1: e.activation(out=sgt[si][:], in_=banks[b1][:], func=AF.Silu),
                                    [b_bank[b1]], [b_sg[si]])
                            sc.emit(dve, lambda e, si=si, b3=b3, fc=fc: e.tensor_tensor(
                                out=hidT[:, fc, :], in0=banks[b3][:], in1=sgt[si][:], op=ALU.mult),
                                [b_bank[b3], b_sg[si]], [b_hidT])
                    mm_tm(hidT, b_hidT, FC, w2, bw2, D, 0.5)

                def inproj_a(l_, tg):
                    W, bW = wbf["w_in"][l_], b_w["w_in"][l_]
                    for (ocol, row, width) in FM_SEGS:
                        tp_pump()
                        pw, bpw = load_panel(W, bW, 0, KC, ocol, width)
                        si = st["stg"] % 2
                        st["stg"] += 1
                        nf = width // 128
                        for f in range(nf):
                            bk = st["bank"] % 4
                            st["bank"] += 1
                            for k in range(KC):
                                mm(banks[bk][:], pw[:, k, f * 128:(f + 1) * 128], xnT[:, k, :], k == 0, k == KC - 1,
                                   [bpw, b_xnT], [b_bank[bk]])
                            if f % 2 == 0:
                                sc.emit(act, lambda e, si=si, f=f, bk=bk: e.copy(out=stg[si][:, f, :], in_=banks[bk][:]),
                                        [b_bank[bk]], [b_stg[si]])
                            else:
                                sc.emit(dve, lambda e, si=si, f=f, bk=bk: e.tensor_copy(out=stg[si][:, f, :], in_=banks[bk][:]),
                                        [b_bank[bk]], [b_stg[si]])
                        dst = featT[row:row + width, tg * 512:(tg + 1) * 512].rearrange("(f p) t -> p f t", p=128)
                        dma_pool(dst, stg[si][:, 0:nf, :], [b_stg[si]], [b_featT])
                    for (ocol, vo, width) in [(O_VA, 0, 512), (O_VB, 512, 512), (O_VC, 1024, 128)]:
                        pw, bpw = load_panel(W, bW, 0, KC, ocol, width)
                        for s in range(4):
                            bk = 4 + s
                            for k in range(KC):
                                mm(banks[bk][:, 0:width], xnT[:, k, s * 128:(s + 1) * 128], pw[:, k, :], k == 0, k == KC - 1,
                                   [bpw, b_xnT], [b_bank[bk]])
                            vi = st["vst"] % 2
                            st["vst"] += 1
                            sc.emit(act, lambda e, vi=vi, bk=bk, width=width: e.copy(
                                out=vst[vi][:, 0:width], in_=banks[bk][:, 0:width]), [b_bank[bk]], [b_vst[vi]])
                            r0 = tg * 512 + s * 128
                            dma_pool(vscr[r0:r0 + 128, vo:vo + width], vst[vi][:, 0:width], [b_vst[vi]], [b_vscr])

                def mixer_post(l_, tg):
                    W, bW = wbf["w_in"][l_], b_w["w_in"][l_]
                    WB, bWB = wbf["w_branch"][l_], b_w["w_branch"][l_]
                    src_ap = yT[:, tg * 512:(tg + 1) * 512].rearrange("(k p) t -> p k t", p=128)
                    dma_pool(hidT[:, 0:16, :], src_ap, [b_yT], [b_hidT])
                    for c0 in range(0, D, 512):
                        for n in range(4):
                            tp_pump()
                            pw, bpw = load_panel(W, bW, 0, KC, O_GATE + n * D + c0, 512)
                            bp, bbp = load_panel(WB, bWB, n * 4, 4, c0, 512)
                            for f in range(4):
                                fc = c0 // 128 + f
                                ba = st["bank"] % 2
                                st["bank"] += 1
                                bg_, bb_ = ba, 2 + ba
                                for k in range(KC):
                                    mm(banks[bg_][:], pw[:, k, f * 128:(f + 1) * 128], xnT[:, k, :], k == 0, k == KC - 1,
                                       [bpw, b_xnT], [b_bank[bg_]])
                                for k in range(4):
                                    mm(banks[bb_][:], bp[:, k, f * 128:(f + 1) * 128], hidT[:, n * 4 + k, :],
                                       k == 0, k == 3, [bbp, b_hidT], [b_bank[bb_]])
                                si = st["sg"] % 2
                                st["sg"] += 1
                                sc.emit(act, lambda e, si=si, bg_=bg_: e.activation(out=sgt[si][:], in_=banks[bg_][:],
                                                                                   func=AF.Sigmoid),
                                        [b_bank[bg_]], [b_sg[si]])
                                if n == 0:
                                    sc.emit(dve, lambda e, si=si, bb_=bb_, f=f: e.tensor_tensor(
                                        out=acc[:, f, :], in0=banks[bb_][:], in1=sgt[si][:], op=ALU.mult),
                                        [b_bank[bb_], b_sg[si]], [b_acc[f]])
                                else:
                                    sc.emit(dve, lambda e, si=si, bb_=bb_: e.tensor_tensor(
                                        out=tmp[:], in0=banks[bb_][:], in1=sgt[si][:], op=ALU.mult),
                                        [b_bank[bb_], b_sg[si]], [b_tmp])
                                    if n < 3:
                                        sc.emit(pool, lambda e, f=f: e.tensor_tensor(out=acc[:, f, :], in0=acc[:, f, :],
                                                                                      in1=tmp[:], op=ALU.add),
                                                [b_acc[f], b_tmp], [b_acc[f]])
                                    else:
                                        sc.emit(pool, lambda e, f=f, fc=fc: e.tensor_tensor(
                                            out=hidT[:, 16 + fc, :], in0=acc[:, f, :], in1=tmp[:], op=ALU.add),
                                            [b_acc[f], b_tmp], [b_hidT])
                    mm_tm(hidT, b_hidT, KC, wbf["w_out"][l_], b_w["w_out"][l_], D, 1.0, k_off=16)

                srcd, bsrc = (x_in, None) if first else (hbuf, b_hbuf)

                def load_h(tg2):
                    for s in range(4):
                        dma_pool(h[:, s, :], srcd[tg2 * 512 + s * 128:tg2 * 512 + (s + 1) * 128, :],
                                 [bsrc] if bsrc else [], [b_h[s]])

                load_h(0)
                for tg in range(NTG):
                    cur["tg"] = tg
                    r0 = tg * 512
                    if last and tg > 0:
                        load_h(tg)
                    if not first:
                        norm_transpose((l - 1) * 3 + 1)
                        mixer_post(l - 1, tg)
                        norm_transpose((l - 1) * 3 + 2)
                        ffn(l - 1, "ffn2")
                    if not last:
                        norm_transpose(l * 3 + 0)
                        ffn(l, "ffn1")
                        for s in range(4):
                            dma_pool(hbuf[r0 + s * 128:r0 + (s + 1) * 128, :], h[:, s, :], [b_h[s]], [b_hbuf])
                        norm_transpose(l * 3 + 1)
                        if first and tg == 0 and cvs["pos"] < n_win0:
                            pump(n_win0 - cvs["pos"], [pool, dve])
                            cv_flush()
                        if tg + 1 < NTG:
                            load_h(tg + 1)
                        inproj_a(l, tg)
                    else:
                        if tg == 0:
                            dma_pool(gbc[:], fnorm[0, :].partition_broadcast(128), [], [b_gbc])
                        for s in range(4):
                            rstd_for(s)
                            sc.emit(dve, lambda e, s=s: e.scalar_tensor_tensor(
                                out=h[:, s, :], in0=h[:, s, :], scalar=ss[:, 2:3], in1=gbc[:],
                                op0=ALU.mult, op1=ALU.mult), [b_h[s], b_ss, b_gbc], [b_h[s]])
                            dma_pool(out_d[r0 + s * 128:r0 + (s + 1) * 128, :], h[:, s, :], [b_h[s]], [b_out])
                if todo > 0:
                    if cvs["pos"] < tgt:
                        pump(tgt - cvs["pos"], [pool, dve])
                    cv_flush()
            sc.barrier()

        def attention_phase(l):
            def load_feat(tile, buf, row, nrows=128, p0=0):
                dma_pool(tile[p0:p0 + nrows, :], featT[row:row + nrows, :], [b_featT], [buf])

            with ExitStack() as es:
                cvbs = [[sb("cvb%d_%d" % (j, i), [128, S], BF16, es) for i in range(3)] for j in range(2)]
                b_cvbs = [[Buf() for _ in range(3)] for _ in range(2)]
                cus = [sb("cu%d" % j, [128, S + 2], F32, es) for j in range(2)]
                cys = [sb("cy%d" % j, [128, S], F32, es) for j in range(2)]
                cyos = [sb("cyo%d" % j, [128, S], BF16, es) for j in range(2)]
                b_cus, b_cys, b_cyos = [Buf(), Buf()], [Buf(), Buf()], [Buf(), Buf()]
                cws = [sb("cw%d" % j, [128, 4], F32, es) for j in range(2)]
                b_cws = [Buf(), Buf()]

                def conv_load(ch):
                    j = ch % 2
                    load_feat(cvbs[j][0], b_cvbs[j][0], R_BG + ch * 128)
                    load_feat(cvbs[j][1], b_cvbs[j][1], R_CG + ch * 128)
                    load_feat(cvbs[j][2], b_cvbs[j][2], R_HD + ch * 128)
                    for k in range(3):
                        dma_pool(cws[j][:, k:k + 1], conv_w[l, k, ch * 128:(ch + 1) * 128].rearrange("(c o) -> c o", o=1),
                                 [], [b_cws[j]])

                conv_load(0)
                for ch in range(4):
                    if ch + 1 < 4:
                        conv_load(ch + 1)
                    j = ch % 2
                    cvb, b_cvb, cu, cy, cyo, cw = cvbs[j], b_cvbs[j], cus[j], cys[j], cyos[j], cws[j]
                    b_cu, b_cy, b_cyo, b_cw = b_cus[j], b_cys[j], b_cyos[j], b_cws[j]
                    sc.emit(dve, lambda e, cu=cu: e.memset(cu[:, 0:2], 0.0), [], [b_cu])
                    sc.emit(dve, lambda e, cu=cu, cvb=cvb: e.tensor_tensor(out=cu[:, 2:S + 2], in0=cvb[1][:], in1=cvb[2][:],
                                                                           op=ALU.mult), [b_cvb[1], b_cvb[2]], [b_cu])
                    sc.emit(dve, lambda e, cu=cu, cy=cy, cw=cw: e.tensor_scalar(
                        out=cy[:], in0=cu[:, 0:S], scalar1=cw[:, 0:1], scalar2=None, op0=ALU.mult), [b_cu, b_cw], [b_cy])
                    sc.emit(dve, lambda e, cu=cu, cy=cy, cw=cw: e.scalar_tensor_tensor(
                        out=cy[:], in0=cu[:, 1:S + 1], scalar=cw[:, 1:2], in1=cy[:], op0=ALU.mult, op1=ALU.add),
                        [b_cu, b_cw, b_cy], [b_cy])
                    sc.emit(dve, lambda e, cu=cu, cy=cy, cw=cw: e.scalar_tensor_tensor(
                        out=cy[:], in0=cu[:, 2:S + 2], scalar=cw[:, 2:3], in1=cy[:], op0=ALU.mult, op1=ALU.add),
                        [b_cu, b_cw, b_cy], [b_cy])
                    sc.emit(dve, lambda e, cy=cy, cyo=cyo, cvb=cvb: e.tensor_tensor(out=cyo[:], in0=cy[:], in1=cvb[0][:],
                                                                                    op=ALU.mult), [b_cy, b_cvb[0]], [b_cyo])
                    dma_pool(yT[1536 + ch * 128:1536 + (ch + 1) * 128, :], cyo[:], [b_cyo], [b_yT])
            sc.barrier()

            with ExitStack() as es:
                b_ac = Buf("attconst")
                cs = {}
                for k_, shape, dt in [("c_mstrict", [128, 128], BF16), ("c_mincl", [128, 128], BF16),
                                      ("c_mpair", [128, 256], BF16), ("c_negu", [128, 128], BF16),
                                      ("c_ind", [128, 160], BF16), ("c_negep", [128, 4096], BF16),
                                      ("c_onesp", [128, 256], BF16), ("c_neg8i", [128, 128], BF16),
                                      ("c_ones128", [128, 128], BF16), ("c_pastneg", [128, 512], F32),
                                      ("c_negbig", [128, 512], F32)]:
                    t_ = sb(k_, shape, dt, es)
                    cs[k_] = t_
                    dma_pool(t_[:], cin[k_][:], [], [b_ac])
                cv_open(es, 3)
                n_iter_pts = 3 * 4 * 2 * NTG
                att_tgt = sum(1 for t in conv_tasks if t[1] <= l or (t[1] == l + 1 and t[0] in WNAMES[:4]))
                remaining = max(0, att_tgt - cvs["pos"])
                per_pt = 0
                w_tot = 2 * 4 * sum(4 * g_ + 4 for g_ in range(NTG)) * 1.6 + 0.2 * 64
                quota = {"acc": 0.0}

                def wpump(w):
                    if remaining <= 0:
                        return
                    quota["acc"] += 1.15 * remaining * w / w_tot
                    k_ = int(quota["acc"])
                    if k_ > 0:
                        quota["acc"] -= k_
                        pump(k_, [pool])
                qzs = [[sb("qz%d_%d" % (i, hh), [128, S], BF16, es) for hh in range(2)] for i in range(2)]
                kTs = [sb("kT%d" % i, [128, S], BF16, es) for i in range(2)]
                vzs = [[sb("vz%d_%d" % (i, hh), [128, NQT, 128], BF16, es) for hh in range(2)] for i in range(2)]
                b_qs, b_ks, b_vs = [Buf(), Buf()], [Buf(), Buf()], [Buf(), Buf()]
                for i in range(2):
                    for hh in range(2):
                        o0 = 64 * (1 - hh)
                        sc.emit(pool, lambda e, t=qzs[i][hh], o0=o0: e.memset(t[o0:o0 + 64, :], 0.0), [], [b_qs[i]])
                        sc.emit(pool, lambda e, t=vzs[i][hh], o0=o0: e.memset(t[:, :, o0:o0 + 64], 0.0), [], [b_vs[i]])
                qaugs = [sb("qaug%d" % i, [128, S], BF16, es) for i in range(2)]
                kaugs = [sb("kaug%d" % i, [128, S], BF16, es) for i in range(2)]
                b_qaugs, b_kaugs = [Buf(), Buf()], [Buf(), Buf()]
                for i in range(2):
                    sc.emit(pool, lambda e, t=qaugs[i]: e.memset(t[:], 0.0), [], [b_qaugs[i]])
                    sc.emit(pool, lambda e, t=kaugs[i]: e.memset(t[:], 0.0), [], [b_kaugs[i]])
                NSP = 4
                sp_sb = [sb("sp_sb%d" % i, [128, 512], BF16, es) for i in range(NSP)]
                b_sps = [Buf() for _ in range(NSP)]
                carry_bf = [sb("carry%d" % i, [128, 512], BF16, es) for i in range(2)]
                b_carry = [Buf() for _ in range(2)]
                zero_bf = sb("zero_bf", [128, 512], BF16, es)
                sc.emit(pool, lambda e: e.memset(zero_bf[:], 0.0), [], [b_ac])
                e_sb = [sb("e_sb%d" % i, [128, 512], F32, es) for i in range(2)]
                b_e = [Buf() for _ in range(2)]
                NA = 4
                a_sb = [sb("a_sb%d" % i, [128, 512], BF16, es) for i in range(NA)]
                b_a = [Buf() for _ in range(NA)]
                tot_sb = sb("tot_sb", [128, 512], BF16, es)
                b_tot = Buf("tot")
                ost = [sb("ost%d" % i, [128, 512], BF16, es) for i in range(2)]
                b_ost = [Buf() for _ in range(2)]
                rden = sb("rden", [128, 512], F32, es)
                b_rden = Buf("rden")
                esink = sb("esink", [128, 2], F32, es)
                b_esink = Buf("esink")
                kmeans = [sb("kmean%d" % i, [128, 16], F32, es) for i in range(2)]
                kmTs = [sb("kmT%d" % i, [128, 16], BF16, es) for i in range(2)]
                b_kms = [Buf(), Buf()]
                gm = sb("gm", [128, 32, 16], F32, es)
                top8 = sb("top8", [128, 32, 8], F32, es)
                selb = sb("selb", [128, 32, 16], BF16, es)
                lt = sb("lt", [128, 32, 16], F32, es)
                b_gm, b_top8, b_selb, b_lt = Buf("gm"), Buf("top8"), Buf("selb"), Buf("lt")
                ctr = {"z": 0, "g": 0, "e": 0, "a": 0, "o": 0, "ost": 0, "sp": 0, "mz": 0}

                def pipeline(items, stage_c, stage_d, la=2, hooks=None):
                    n_ = len(items)
                    for i in range(n_ + la):
                        if hooks and i in hooks:
                            hooks[i]()
                        if i < n_:
                            stage_c(items[i])
                        if i - la >= 0:
                            stage_d(items[i - la])

                def qk_cols(g, ks):
                    col0 = max(0, ks - 4 * g) * 128
                    return col0, 512 - col0

                pair_jobs = [("sb", p) for p in range(4)] + [("moba", p) for p in range(4)] + [("swa", p) for p in range(4)]

                def load_pair(ji):
                    kind, p = pair_jobs[ji]
                    i = ji % 2
                    if kind == "sb":
                        qrow, krow, vcol = R_QB + p * 128, R_KB + p * 128, 512 + p * 128
                    elif kind == "moba":
                        qrow, krow, vcol = R_QA + p * 128, R_KA + p * 128, p * 128
                    else:
                        qrow, vcol = R_QC + p * 128, 1024 + (p // 2) * 64
                    for hh in range(2):
                        dma_pool(qzs[i][hh][64 * hh:64 * hh + 64, :], featT[qrow + 64 * hh:qrow + 64 * hh + 64, :],
                                 [b_featT], [b_qs[i]])
                    if kind == "swa":
                        kr = R_KC + (p // 2) * 64
                        dma_pool(kTs[i][0:64, :], featT[kr:kr + 64, :], [b_featT], [b_ks[i]])
                        dma_pool(kTs[i][64:128, :], featT[kr:kr + 64, :], [b_featT], [b_ks[i]])
                    else:
                        dma_pool(kTs[i][:, :], featT[krow:krow + 128, :], [b_featT], [b_ks[i]])
                    for hh in range(2):
                        vc = vcol if kind == "swa" else vcol + 64 * hh
                        src_ap = vscr[:, vc:vc + 64].rearrange("(t p) c -> p t c", p=128)
                        dma_pool(vzs[i][hh][:, :, 64 * hh:64 * hh + 64], src_ap, [b_vscr], [b_vs[i]])

                def evac_norm_store(nb_, db_, base, row, g, add_sink):
                    if add_sink:
                        sc.emit(act, lambda e: e.activation(
                            out=rden[base:base + 64, :], in_=banks[db_][base:base + 64, :], func=AF.Ln,
                            bias=esink[base:base + 64, 1:2], scale=1.0), [b_bank[db_], b_esink], [b_rden])
                        sc.emit(act, lambda e: e.activation(
                            out=rden[base:base + 64, :], in_=rden[base:base + 64, :], func=AF.Exp, scale=-1.0),
                            [b_rden], [b_rden])
                    else:
                        sc.emit(dve, lambda e: e.reciprocal(out=rden[base:base + 64, :], in_=banks[db_][base:base + 64, :]),
                                [b_bank[db_]], [b_rden])
                    si = ctr["ost"] % 2
                    ctr["ost"] += 1
                    sc.emit(dve, lambda e: e.tensor_tensor(
                        out=ost[si][base:base + 64, :], in0=banks[nb_][base:base + 64, :],
                        in1=rden[base:base + 64, :], op=ALU.mult), [b_bank[nb_], b_rden], [b_ost[si]])
                    dma_pool(yT[row + base:row + base + 64, g * 512:(g + 1) * 512],
                             ost[si][base:base + 64, :], [b_ost[si]], [b_yT])

                load_pair(0)
                for ji, (kind, p) in enumerate(pair_jobs):
                    if ji + 1 < len(pair_jobs):
                        load_pair(ji + 1)
                    pi = ji % 2
                    kT = kTs[pi]
                    b_q, b_k, b_v = b_qs[pi], b_ks[pi], b_vs[pi]

                    if kind == "sb":
                        for g in range(NTG):
                            kmax = 4 * g + 3
                            ob, tb = 5, 4
                            for hh in range(2):
                                base = 64 * hh
                                qz, vz = qzs[pi][hh], vzs[pi][hh]
                                info = {}
                                mm(banks[tb][:], cs["c_ones128"][:], zero_bf[:], True, False, [b_ac], [b_bank[tb]])
                                mm(banks[ob][:], cs["c_ones128"][:], zero_bf[:], True, False, [b_ac], [b_bank[ob]])

                                def s1(ks, qz=qz, kT=kT, g=g, info=info):
                                    col0, n = qk_cols(g, ks)
                                    diag = ks >= 4 * g
                                    bk = ctr["z"] % 4
                                    ctr["z"] += 1
                                    mm(banks[bk][:, col0:512], kT[:, ks * 128:(ks + 1) * 128],
                                       qz[:, g * 512 + col0:(g + 1) * 512], True, False, [b_k, b_q], [b_bank[bk]])
                                    if diag:
                                        mm(banks[bk][:, col0:col0 + 128], ident[:], cs["c_mstrict"][:], False, False,
                                           [b_const, b_ac], [b_bank[bk]])
                                    ei = ctr["e"] % 2
                                    ctr["e"] += 1
                                    si_ = ctr["sp"] % NSP
                                    ctr["sp"] += 1
                                    info[ks] = (bk, col0, si_)
                                    sc.emit(act, lambda e, bk=bk, ei=ei, col0=col0: e.activation(
                                        out=e_sb[ei][:, col0:512], in_=banks[bk][:, col0:512], func=AF.Exp, scale=0.125),
                                        [b_bank[bk]], [b_e[ei]])
                                    sc.emit(act, lambda e, ei=ei, si_=si_, col0=col0: e.activation(
                                        out=sp_sb[si_][:, col0:512], in_=e_sb[ei][:, col0:512], func=AF.Ln,
                                        scale=1.0, bias=1.0), [b_e[ei]], [b_sps[si_]])

                                def s2(ks, kmax=kmax, info=info, tb=tb):
                                    bk, col0, si_ = info[ks]
                                    first = ks == kmax
                                    mm(banks[bk][:, col0:512], cs["c_negu"][:], sp_sb[si_][:, col0:512], False, first,
                                       [b_sps[si_], b_ac], [b_bank[bk]])
                                    if not first:
                                        ci_ = (ks + 1) % 2
                                        mm(banks[bk][:, col0:512], cs["c_neg8i"][:], carry_bf[ci_][:, col0:512], False, True,
                                           [b_carry[ci_], b_ac], [b_bank[bk]])
                                    if ks > 0:
                                        mm(banks[tb][:, col0:512], cs["c_ones128"][:], sp_sb[si_][:, col0:512], False, False,
                                           [b_sps[si_], b_ac], [b_bank[tb]])
                                        co_ = ks % 2
                                        sc.emit(dve, lambda e, co_=co_, tb=tb: e.tensor_copy(out=carry_bf[co_][:], in_=banks[tb][:]),
                                                [b_bank[tb]], [b_carry[co_]])
                                    ai = ctr["a"] % NA
                                    ctr["a"] += 1
                                    info[ks] = (bk, col0, si_, ai)
                                    sc.emit(act, lambda e, bk=bk, ai=ai, col0=col0: e.activation(
                                        out=a_sb[ai][:, col0:512], in_=banks[bk][:, col0:512], func=AF.Exp, scale=0.125),
                                        [b_bank[bk]], [b_a[ai]])

                                def s3(ks, info=info, vz=vz, ob=ob):
                                    bk, col0, si_, ai = info[ks]
                                    mm(banks[ob][:, col0:512], vz[:, ks, :], a_sb[ai][:, col0:512], False, ks == 0,
                                       [b_v, b_a[ai]], [b_bank[ob]])

                                order = list(range(kmax, -1, -1))
                                n_ = len(order)
                                for i in range(n_ + 2):
                                    if i < n_:
                                        s1(order[i])
                                    if 0 <= i - 1 < n_:
                                        s2(order[i - 1])
                                    if 0 <= i - 2 < n_:
                                        s3(order[i - 2])
                                si = ctr["ost"] % 2
                                ctr["ost"] += 1
                                sc.emit(dve, lambda e, si=si, ob=ob, base=base: e.tensor_copy(
                                    out=ost[si][base:base + 64, :], in_=banks[ob][base:base + 64, :]),
                                    [b_bank[ob]], [b_ost[si]])
                                r_ = 512 + p * 128 + base
                                dma_pool(yT[r_:r_ + 64, g * 512:(g + 1) * 512], ost[si][base:base + 64, :],
                                         [b_ost[si]], [b_yT])
                                wpump(4 * g + 4)

                    elif kind == "moba":
                        nq16 = NQT * 16
                        GB = 3
                        moba_heads = [(ji2, p2, hh2) for ji2, (k2, p2) in enumerate(pair_jobs) if k2 == "moba" for hh2 in range(2)]

                        def prep_a(m):
                            ji2, p2, hh2 = moba_heads[m]
                            pi2 = ji2 % 2
                            kT2, qz2 = kTs[pi2], qzs[pi2][hh2]
                            kmean, kmT, b_km = kmeans[p2 % 2], kmTs[p2 % 2], b_kms[p2 % 2]
                            if hh2 == 0:
                                sc.emit(dve, lambda e, kT2=kT2, kmean=kmean: e.tensor_reduce(
                                    out=kmean[:, 0:NBLK], in_=kT2[:, :].rearrange("p (n j) -> p n j", j=256),
                                    axis=AX.X, op=ALU.add), [b_ks[pi2]], [b_km])
                                if NBLK < 16:
                                    sc.emit(dve, lambda e, kmT=kmT: e.memset(kmT[:, NBLK:16], 0.0), [], [b_km])
                                sc.emit(dve, lambda e, kmT=kmT, kmean=kmean: e.tensor_scalar(
                                    out=kmT[:, 0:NBLK], in0=kmean[:, 0:NBLK], scalar1=1.0 / 256.0,
                                    scalar2=None, op0=ALU.mult), [b_km], [b_km])
                            hd2 = 2 * p2 + hh2
                            qaug, kaug, b_qaug, b_kaug = qaugs[hh2], kaugs[hh2], b_qaugs[hh2], b_kaugs[hh2]
                            dma_pool(qaug[16:20, :], cin["c_qaug_a"][hd2], [], [b_qaug])
                            dma_pool(kaug[0:20, :], cin["c_kaug_a"][hd2], [], [b_kaug])
                            for tq in range(NQT):
                                mm(banks[GB][:, tq * 16:(tq + 1) * 16], qz2[:, tq * 128:(tq + 1) * 128],
                                   kmT[:, :], True, True, [b_qs[pi2], b_km], [b_bank[GB]])
                            gflat = gm[:].rearrange("p a b -> p (a b)")
                            sc.emit(dve, lambda e: e.tensor_tensor(out=gflat[:, 0:nq16], in0=banks[GB][:, 0:nq16],
                                                                   in1=cs["c_pastneg"][:, 0:nq16], op=ALU.add),
                                    [b_bank[GB], b_ac], [b_gm])
                            for tq in range(NQT):
                                sc.emit(dve, lambda e, tq=tq: e.max(out=top8[:, tq, :], in_=gm[:, tq, :]), [b_gm], [b_top8])
                            sc.emit(dve, lambda e: e.tensor_tensor(
                                out=lt[:, 0:NQT, :], in0=gm[:, 0:NQT, :],
                                in1=top8[:, 0:NQT, 2:3].broadcast_to([128, NQT, 16]), op=ALU.is_lt),
                                [b_gm, b_top8], [b_lt])
                            sc.emit(dve, lambda e: e.tensor_tensor(
                                out=selb[:, 0:NQT, :], in0=lt[:, 0:NQT, :],
                                in1=cs["c_negbig"][:, 0:nq16].rearrange("p (a b) -> p a b", b=16), op=ALU.mult),
                                [b_lt, b_ac], [b_selb])

                        def prep_b(m):
                            ji2, p2, hh2 = moba_heads[m]
                            qaug, b_qaug = qaugs[hh2], b_qaugs[hh2]
                            for t8 in range(0, NQT, 8):
                                pv = banks[GB][:].bitcast(BF16)
                                for jj in range(8):
                                    tq = t8 + jj
                                    sc.emit(pe, lambda e, pv=pv, jj=jj, tq=tq: e.transpose(
                                        pv[0:16, jj * 128:(jj + 1) * 128], selb[:, tq, :], ident[:]),
                                        [b_selb, b_const], [b_bank[GB]])
                                sc.emit(act, lambda e, pv=pv, t8=t8, qaug=qaug: e.copy(
                                    out=qaug[0:16, t8 * 128:(t8 + 8) * 128], in_=pv[0:16, 0:1024]),
                                    [b_bank[GB]], [b_qaug])

                        for hh in range(2):
                            m = moba_heads.index((ji, p, hh))
                            if m == 0:
                                prep_a(0)
                                prep_b(0)
                            base = 64 * hh
                            qz, vz = qzs[pi][hh], vzs[pi][hh]
                            qaug, kaug, b_qaug, b_kaug = qaugs[hh], kaugs[hh], b_qaugs[hh], b_kaugs[hh]
                            nbs = {}

                            def mo_c(item, qz=qz, kT=kT, nbs=nbs, qaug=qaug, kaug=kaug, b_qaug=b_qaug, b_kaug=b_kaug):
                                g, ks = item
                                bk = ctr["mz"] % 3
                                ctr["mz"] += 1
                                col0, n = qk_cols(g, ks)
                                diag = ks >= 4 * g
                                mm(banks[bk][:, col0:512], kT[:, ks * 128:(ks + 1) * 128],
                                   qz[:, g * 512 + col0:(g + 1) * 512], True, False,
                                   [b_k, b_q], [b_bank[bk]])
                                mm(banks[bk][:, col0:512], kaug[:, ks * 128:(ks + 1) * 128],
                                   qaug[:, g * 512 + col0:(g + 1) * 512], False, not diag,
                                   [b_kaug, b_qaug], [b_bank[bk]])
                                if diag:
                                    mm(banks[bk][:, col0:col0 + 128], ident[:], cs["c_mincl"][:], False, True,
                                       [b_const, b_ac], [b_bank[bk]])
                                ai = ctr["a"] % NA
                                ctr["a"] += 1
                                nbs[item] = ai
                                sc.emit(act, lambda e, bk=bk, ai=ai, col0=col0: e.activation(
                                    out=a_sb[ai][:, col0:512], in_=banks[bk][:, col0:512], func=AF.Exp, scale=0.125),
                                    [b_bank[bk]], [b_a[ai]])

                            def mo_d(item, base=base, p=p, vz=vz, hh=hh, nbs=nbs):
                                g, ks = item
                                kmax = 4 * g + 3
                                col0, n = qk_cols(g, ks)
                                ai = nbs[item]
                                nb_ = 4 + (g % 2)
                                db_ = 6 + (g % 2)
                                mm(banks[nb_][:, col0:512], vz[:, ks, :], a_sb[ai][:, col0:512],
                                   ks == 0, ks == kmax, [b_v, b_a[ai]], [b_bank[nb_]])
                                mm(banks[db_][:, col0:512], cs["c_onesp"][:, hh * 128:(hh + 1) * 128], a_sb[ai][:, col0:512],
                                   ks == 0, ks == kmax, [b_ac, b_a[ai]], [b_bank[db_]])
                                if ks == kmax:
                                    evac_norm_store(nb_, db_, base, p * 128, g, False)
                                    wpump(0.6 * (4 * g + 4))

                            items = [(g, ks) for g in range(NTG) for ks in range(4 * g + 4)]
                            hooks = None
                            if m + 1 < len(moba_heads):
                                hooks = {len(items) // 2: (lambda m=m: prep_a(m + 1)),
                                         (3 * len(items)) // 4: (lambda m=m: prep_b(m + 1))}
                            pipeline(items, mo_c, mo_d, la=2, hooks=hooks)

                    else:
                        if p == 0:
                            for i_ in range(2):
                                sc.emit(pool, lambda e, t=qaugs[i_]: e.memset(t[0:16, :], 0.0), [], [b_qaugs[i_]])
                        dma_pool(esink[0:64, 0:1], sinks[l, 2 * p:2 * p + 1].partition_broadcast(64), [], [b_esink])
                        dma_pool(esink[64:128, 0:1], sinks[l, 2 * p + 1:2 * p + 2].partition_broadcast(64), [], [b_esink])
                        sc.emit(act, lambda e: e.activation(out=esink[:, 1:2], in_=esink[:, 0:1], func=AF.Exp),
                                [b_esink], [b_esink])
                        for hh in range(2):
                            hd_ = 2 * p + hh
                            base = 64 * hh
                            qz, vz = qzs[pi][hh], vzs[pi][hh]
                            qaug, kaug, b_qaug, b_kaug = qaugs[hh], kaugs[hh], b_qaugs[hh], b_kaugs[hh]
                            dma_pool(qaug[16:20, :], cin["c_qaug_c"][hd_], [], [b_qaug])
                            dma_pool(kaug[16:20, :], cin["c_kaug_c"][hd_], [], [b_kaug])
                            nbs = {}

                            def sw_c(tq, qz=qz, kT=kT, nbs=nbs, qaug=qaug, kaug=kaug, b_qaug=b_qaug, b_kaug=b_kaug):
                                bk = ctr["z"] % 4
                                ctr["z"] += 1
                                c_lo = 0 if tq > 0 else 128
                                qs = slice(tq * 128, (tq + 1) * 128)
                                ksl = [ks for ks in (tq - 1, tq) if ks >= 0]
                                for ks in ksl:
                                    w_ = ks - (tq - 1)
                                    ws = slice(w_ * 128, (w_ + 1) * 128)
                                    mm(banks[bk][:, ws], kT[:, ks * 128:(ks + 1) * 128],
                                       qz[:, qs], True, False, [b_k, b_q], [b_bank[bk]])
                                    mm(banks[bk][:, ws], kaug[:, ks * 128:(ks + 1) * 128],
                                       qaug[:, qs], False, False, [b_kaug, b_qaug], [b_bank[bk]])
                                    mm(banks[bk][:, ws], ident[:], cs["c_mpair"][:, ws], False, True,
                                       [b_const, b_ac], [b_bank[bk]])
                                ai = ctr["a"] % NA
                                ctr["a"] += 1
                                nbs[tq] = ai
                                sc.emit(act, lambda e, bk=bk, ai=ai, c_lo=c_lo: e.activation(
                                    out=a_sb[ai][:, c_lo:256], in_=banks[bk][:, c_lo:256], func=AF.Exp, scale=0.125),
                                    [b_bank[bk]], [b_a[ai]])

                            def sw_d(tq, base=base, p=p, vz=vz, hh=hh, nbs=nbs):
                                g, t4 = tq // 4, tq % 4
                                ai = nbs[tq]
                                nb_ = 4 + (g % 2)
                                db_ = 6 + (g % 2)
                                ksl = [ks for ks in (tq - 1, tq) if ks >= 0]
                                for idx, ks in enumerate(ksl):
                                    w_ = ks - (tq - 1)
                                    mm(banks[nb_][:, t4 * 128:(t4 + 1) * 128], vz[:, ks, :],
                                       a_sb[ai][:, w_ * 128:(w_ + 1) * 128], idx == 0, idx == len(ksl) - 1,
                                       [b_v, b_a[ai]], [b_bank[nb_]])
                                for idx, ks in enumerate(ksl):
                                    w_ = ks - (tq - 1)
                                    mm(banks[db_][:, t4 * 128:(t4 + 1) * 128], cs["c_onesp"][:, hh * 128:(hh + 1) * 128],
                                       a_sb[ai][:, w_ * 128:(w_ + 1) * 128], idx == 0, idx == len(ksl) - 1,
                                       [b_ac, b_a[ai]], [b_bank[db_]])
                                if t4 == 3:
                                    evac_norm_store(nb_, db_, base, 1024 + p * 128, g, True)
                                    wpump(0.2)

                            pipeline(list(range(NQT)), sw_c, sw_d, la=2)
                if cvs["pos"] < att_tgt:
                    pump(att_tgt - cvs["pos"], [dve, pool])
                cv_flush()
            sc.barrier()

        for l in range(L + 1):
            token_phase(first=(l == 0), last=(l == L), l=l)
            if l < L:
                attention_phase(l)

        sc.emit(sp, lambda e: e.nop(), [b_out], [])

        with nc.Block() as block:
            @block.tensor
            def _(e):
                sc.replay(sc.pe, e)

            @block.scalar
            def _(e):
                sc.replay(sc.act, e)

            @block.vector
            def _(e):
                sc.replay(sc.dve, e)

            @block.gpsimd
            def _(e):
                sc.replay(sc.pool, e)

            @block.sync
            def _(e):
                sc.replay(sc.sp, e)
    return nc, consts


def make_in_maps(inputs, consts, L, nb):
    x = np.ascontiguousarray(inputs["x"], dtype=np.float32)
    shared = {}
    for n in WNAMES:
        w = np.asarray(inputs[n], dtype=np.float32)
        shared[n] = np.ascontiguousarray(w.reshape((L,) + WSHAPES[n]))
    for n in ["ffn1_norm", "mix_norm", "ffn2_norm"]:
        shared[n] = np.ascontiguousarray(inputs[n], dtype=np.float32)
    shared["final_norm"] = np.ascontiguousarray(np.asarray(inputs["final_norm"], np.float32).reshape(1, D))
    shared["conv_w"] = np.ascontiguousarray(np.asarray(inputs["conv_w"], np.float32).reshape(L, 3, 512))
    shared["attn_sinks"] = np.ascontiguousarray(inputs["attn_sinks"], dtype=np.float32)
    shared.update(consts)
    return [dict(shared, x=x[b]) for b in range(nb)]


_CACHE = {}


def kernel(**inputs):
    S, L = 4096, 2
    if "prog" not in _CACHE:
        _CACHE["prog"] = build(S, L)
    nc, consts = _CACHE["prog"]
    B = np.asarray(inputs["x"]).shape[0]
    in_maps = make_in_maps(inputs, consts, L, B)
    res = run_bass_kernel_spmd(nc, in_maps, core_ids=list(range(B)))
    return np.stack([np.asarray(r["out"]) for r in res.results], axis=0).astype(np.float32)
```

```python
import numpy as np
import concourse.bass as bass
import concourse.mybir as mybir
from concourse.bass_utils import run_bass_kernel_spmd

F32 = mybir.dt.float32
BF16 = mybir.dt.bfloat16
AF = mybir.ActivationFunctionType
ALU = mybir.AluOpType
AX = mybir.AxisListType

D = 2048
DFF = 5632
KC = D // 128
FC = DFF // 128
EPS = 1e-6
NEG = -32768.0
O_QA, O_KA, O_VA, O_QB, O_KB, O_VB, O_QC, O_KC, O_VC, O_BG, O_CG, O_HD, O_GATE = (
    0, 512, 1024, 1536, 2048, 2560, 3072, 3584, 3712, 3840, 4352, 4864, 5376)
IN_COLS = 13568
R_QA, R_KA, R_QB, R_KB, R_QC, R_KC, R_BG, R_CG, R_HD = 0, 512, 1024, 1536, 2048, 2560, 2688, 3200, 3712
FEAT_ROWS = 4224
FM_SEGS = [(O_QA, R_QA, 512), (O_KA, R_KA, 512), (O_QB, R_QB, 512), (O_KB, R_KB, 512), (O_QC, R_QC, 512),
           (O_KC, R_KC, 128), (O_BG, R_BG, 512), (O_CG, R_CG, 512), (O_HD, R_HD, 512)]
VW = 1152


class Src:
    def __init__(self, name, k, inc):
        self.name, self.k, self.inc = name, k, inc
        self.count = 0
        self.sems = None


class Stream:
    def __init__(self, name, inorder=False):
        self.name = name
        self.ops = []
        self.seen = {}
        self.inorder = inorder
        self.src = Src(name, 1, 1)


class Buf:
    __slots__ = ("w", "r", "name")

    def __init__(self, name=""):
        self.w = {}
        self.r = {}
        self.name = name


class Sched:
    def __init__(self):
        self.pe = Stream("pe", inorder=True)
        self.act = Stream("act")
        self.dve = Stream("dve")
        self.pool = Stream("pool")
        self.sp = Stream("sp")
        self.streams = [self.pe, self.act, self.dve, self.pool, self.sp]
        self.q_sp = Src("qsp", 12, 16)
        self.q_pool = Src("qpool", 8, 16)
        self.srcs = [s.src for s in self.streams] + [self.q_sp, self.q_pool]

    def _wait(self, st, ev):
        src, slot, val = ev
        if src is st.src and st.inorder:
            return
        key = (id(src), slot)
        if st.seen.get(key, 0) < val:
            st.ops.append(("w", src, slot, val))
            st.seen[key] = val

    def emit(self, st, fn, reads=(), writes=(), q=None):
        deps = []
        for b in reads:
            deps.extend((s, sl, v) for (s, sl), v in b.w.values())
        for b in writes:
            deps.extend((s, sl, v) for (s, sl), v in b.w.values())
            deps.extend((s, sl, v) for (s, sl), v in b.r.values())
        for ev in deps:
            self._wait(st, ev)
        src = q if q is not None else st.src
        if src.k > 1:
            i = src.count
            slot = i % src.k
            val = 16 * (i // src.k + 1)
            if i >= src.k:
                self._wait(st, (src, slot, val - 16))
            src.count += 1
        else:
            src.count += 1
            slot, val = 0, src.count
        st.ops.append(("op", fn, src, slot))
        ev = (src, slot, val)
        for b in reads:
            key = (id(src), slot)
            old = b.r.get(key)
            if old is None or old[1] < val:
                b.r[key] = ((src, slot), val)
        for b in writes:
            b.w[(id(src), slot)] = ((src, slot), val)
            b.r = {}
        return ev

    def barrier(self):
        evs = []
        for src in self.srcs:
            if src.count == 0:
                continue
            if src.k == 1:
                evs.append((src, 0, src.count))
            else:
                for slot in range(min(src.k, src.count)):
                    i_last = ((src.count - 1 - slot) // src.k) * src.k + slot
                    evs.append((src, slot, 16 * (i_last // src.k + 1)))
        for st in self.streams:
            for ev in evs:
                self._wait(st, ev)

    def replay(self, st, eng):
        for op in st.ops:
            if op[0] == "w":
                _, src, slot, val = op
                eng.wait_ge(src.sems[slot], val)
            else:
                _, fn, src, slot = op
                fn(eng).then_inc(src.sems[slot], src.inc)


def _bf(x):
    import ml_dtypes
    return np.asarray(x, np.float32).astype(ml_dtypes.bfloat16).astype(np.float32)


def make_consts(S):
    nqt = S // 128
    j = np.arange(128)[:, None]
    i = np.arange(128)[None, :]
    c = {}
    c["c_ident"] = np.eye(128, dtype=np.float32)
    c["c_mstrict"] = np.where(j < i, 0.0, NEG).astype(np.float32)
    c["c_mincl"] = np.where(j <= i, 0.0, NEG).astype(np.float32)
    c["c_mpair"] = np.concatenate([np.where(j > i, 0.0, NEG), np.where(j <= i, 0.0, NEG)], axis=1).astype(np.float32)
    c["c_negu"] = np.where(j >= i, -8.0, 0.0).astype(np.float32)
    ind = np.zeros((128, 160), np.float32)
    ind[:, 31] = 1.0
    c["c_ind"] = ind
    nep = np.zeros((128, 32, 128), np.float32)
    for ks in range(32):
        nep[ks + 1:32, ks, :] = -8.0
    c["c_negep"] = nep.reshape(128, 4096)
    onesp = np.zeros((128, 256), np.float32)
    onesp[:, 0:64] = 1.0
    onesp[:, 128 + 64:256] = 1.0
    c["c_onesp"] = onesp
    c["c_neg8i"] = (-8.0 * np.eye(128)).astype(np.float32)
    c["c_ones128"] = np.ones((128, 128), np.float32)
    tq = np.arange(32)[:, None]
    n = np.arange(16)[None, :]
    past = (n < (tq // 2))
    c["c_pastneg"] = np.broadcast_to(np.where(past, 0.0, -1e30).reshape(1, 512), (128, 512)).astype(np.float32).copy()
    c["c_negbig"] = np.broadcast_to(np.where(past, NEG, 0.0).reshape(1, 512), (128, 512)).astype(np.float32).copy()
    slopes = 2.0 ** (-8.0 * np.arange(1, 17, dtype=np.float64) / 16.0)
    t = np.arange(S)
    ti, tt = (t % 128).astype(np.float32), ((t // 128) * 128).astype(np.float32)
    blk = t // 256

    def aug(sl, with_sel):
        s8 = 8.0 * float(_bf(sl))
        q = np.stack([ti, tt, np.full(S, s8, np.float32), np.full(S, s8, np.float32)]).astype(np.float32)
        k = np.stack([np.full(S, -s8, np.float32), np.full(S, -s8, np.float32), ti, tt]).astype(np.float32)
        if with_sel:
            sel = (blk[None, :] == np.arange(16)[:, None]).astype(np.float32)
            k = np.concatenate([sel, k], axis=0)
        return q, k

    qa, ka, qc, kc = [], [], [], []
    for h in range(8):
        q, k = aug(slopes[8 + h], True)
        qa.append(q); ka.append(k)
        q, k = aug(slopes[h], False)
        qc.append(q); kc.append(k)
    c["c_qaug_a"] = np.stack(qa)
    c["c_kaug_a"] = np.stack(ka)
    c["c_qaug_c"] = np.stack(qc)
    c["c_kaug_c"] = np.stack(kc)
    return c


WNAMES = ["ffn1_w1", "ffn1_w3", "ffn1_w2", "w_in", "w_branch", "w_out", "ffn2_w1", "ffn2_w3", "ffn2_w2"]
WSHAPES = {"ffn1_w1": (D, DFF), "ffn1_w3": (D, DFF), "ffn1_w2": (DFF, D), "w_in": (D, IN_COLS),
           "w_branch": (4 * 512, D), "w_out": (D, D), "ffn2_w1": (D, DFF), "ffn2_w3": (D, DFF), "ffn2_w2": (DFF, D)}


NORM_IDX = {"ffn1_norm": 0, "mix_norm": 1, "ffn2_norm": 2}


def build(S=4096, L=2, debug=False):
    from contextlib import ExitStack
    assert S % 512 == 0
    NTG, NQT, NBLK = S // 512, S // 128, S // 256
    nc = bass.Bass("TRN2", target_bir_lowering=False)
    sc = Sched()
    consts = make_consts(S)

    def din(name, shape, dt=F32):
        return nc.dram_tensor(name, list(shape), dt, kind="ExternalInput").ap()

    x_in = din("x", [S, D])
    out_d = nc.dram_tensor("out", [S, D], F32, kind="ExternalOutput").ap()
    wsrc = {n: din(n, (L,) + WSHAPES[n]) for n in WNAMES}
    norms = {n: din(n, [L, D]) for n in ["ffn1_norm", "mix_norm", "ffn2_norm"]}
    fnorm = din("final_norm", [1, D])
    conv_w = din("conv_w", [L, 3, 512])
    sinks = din("attn_sinks", [L, 8])
    cin = {k: din(k, v.shape) for k, v in consts.items()}
    skind = "ExternalOutput" if debug else "Internal"
    wbf = {n: nc.dram_tensor("s_" + n, [L] + list(WSHAPES[n]), BF16, kind="Internal").ap() for n in WNAMES}
    hbuf = nc.dram_tensor("s_h", [S, D], F32, kind=skind).ap()
    featT = nc.dram_tensor("s_featT", [FEAT_ROWS, S], BF16, kind=skind).ap()
    vscr = nc.dram_tensor("s_v", [S, VW], BF16, kind=skind).ap()
    yT = nc.dram_tensor("s_yT", [D, S], BF16, kind=skind).ap()
    b_hbuf, b_featT, b_vscr, b_yT = Buf("hbuf"), Buf("featT"), Buf("vscr"), Buf("yT")
    if debug:
        d_gm = nc.dram_tensor("d_gm", [128, 512], F32, kind="ExternalOutput").ap()
        d_top8 = nc.dram_tensor("d_top8", [128, 256], F32, kind="ExternalOutput").ap()
        d_lt = nc.dram_tensor("d_lt", [128, 512], F32, kind="ExternalOutput").ap()
        d_km = nc.dram_tensor("d_km", [128, 16], F32, kind="ExternalOutput").ap()
        b_dbg = Buf("dbg")
    b_w = {n: [Buf(n + str(l)) for l in range(L)] for n in WNAMES}
    b_out = Buf("out")

    with ExitStack() as top:
        uid = [0]

        def sb(name, shape, dt, es=top):
            uid[0] += 1
            return es.enter_context(nc.sbuf_tensor("t%d_%s" % (uid[0], name), list(shape), dt))

        banks = [top.enter_context(nc.psum_tensor("bank%d" % i, [128, 512], F32)) for i in range(8)]
        b_bank = [Buf("bank%d" % i) for i in range(8)]
        for s_ in sc.srcs:
            s_.sems = [top.enter_context(nc.semaphore("%s_%d" % (s_.name, i))) for i in range(s_.k)]

        pe, act, dve, pool, sp = sc.pe, sc.act, sc.dve, sc.pool, sc.sp

        def dma_sp(out, in_, reads, writes):
            sc.emit(sp, lambda e: e.dma_start(out=out, in_=in_), reads, writes, q=sc.q_sp)

        def dma_pool(out, in_, reads, writes):
            sc.emit(pool, lambda e: e.dma_start(out=out, in_=in_), reads, writes, q=sc.q_pool)

        def mm(out, lhsT, rhs, start, stop, reads, writes):
            sc.emit(pe, lambda e: e.matmul(out, lhsT, rhs, start=start, stop=stop), reads, writes)

        b_const = Buf("const")
        ident = sb("c_ident", [128, 128], BF16)
        dma_pool(ident[:], cin["c_ident"][:], [], [b_const])
        gT = sb("gT", [128, 3 * L, KC], F32)
        for l in range(L):
            for n, ni in NORM_IDX.items():
                for k in range(KC):
                    dma_pool(gT[:, l * 3 + ni, k:k + 1],
                             norms[n][l, k * 128:(k + 1) * 128].rearrange("(c o) -> c o", o=1), [], [b_const])

        conv_tasks = []
        for l in range(L):
            for n in WNAMES:
                K_, N_ = WSHAPES[n]
                nch = -(-N_ // 1408)
                while N_ % nch:
                    nch += 1
                cw_ = N_ // nch
                for r0 in range(0, K_, 128):
                    for ci in range(nch):
                        conv_tasks.append((n, l, r0, ci * cw_, cw_))
        cvs = {"pos": 0, "pend": [], "bufs": None, "it": 0}

        def cv_open(es, nb=4):
            fin = [sb("cv_in%d" % i, [128, 1408], F32, es) for i in range(nb)]
            fout = [sb("cv_out%d" % i, [128, 1408], BF16, es) for i in range(nb)]
            cvs["bufs"] = (fin, fout, [Buf() for _ in range(nb)], [Buf() for _ in range(nb)], nb)

        def cv_flush():
            for (dst_ap, o_, bfo, bw_) in cvs["pend"]:
                dma_sp(dst_ap, o_, [bfo], [bw_])
            cvs["pend"] = []

        def pump(k, engines):
            fin, fout, b_fin, b_fout, nb = cvs["bufs"]
            for _ in range(k):
                if cvs["pos"] >= len(conv_tasks):
                    break
                n, l, r0, c0, cw = conv_tasks[cvs["pos"]]
                cvs["pos"] += 1
                i = cvs["it"] % nb
                cvs["it"] += 1
                src_ap = wsrc[n][l, r0:r0 + 128, c0:c0 + cw]
                dst_ap = wbf[n][l, r0:r0 + 128, c0:c0 + cw]
                dma_sp(fin[i][:, :cw], src_ap, [], [b_fin[i]])
                o_, a_ = fout[i][:, :cw], fin[i][:, :cw]
                eng = engines[cvs["it"] % len(engines)]
                if eng is act:
                    sc.emit(act, lambda e, o=o_, a=a_: e.copy(out=o, in_=a), [b_fin[i]], [b_fout[i]])
                else:
                    sc.emit(eng, lambda e, o=o_, a=a_: e.tensor_copy(out=o, in_=a), [b_fin[i]], [b_fout[i]])
                cvs["pend"].append((dst_ap, o_, b_fout[i], b_w[n][l]))
                while len(cvs["pend"]) > 2:
                    dst2, o2, bfo2, bw2 = cvs["pend"].pop(0)
                    dma_sp(dst2, o2, [bfo2], [bw2])

        def pump_until(names_layers, engines):
            need = set(names_layers)
            last = -1
            for idx, t in enumerate(conv_tasks):
                if (t[0], t[1]) in need:
                    last = idx
            if last >= cvs["pos"]:
                pump(last + 1 - cvs["pos"], engines)
            cv_flush()

        with ExitStack() as es:
            cv_open(es, 6)
            pump_until([("ffn1_w1", 0), ("ffn1_w3", 0), ("ffn1_w2", 0), ("w_in", 0)], [dve, act, pool])
        sc.barrier()

        def token_phase(first, last, l):
            with ExitStack() as es:
                h = sb("h", [128, 4, D], F32, es)
                b_h = [Buf("h%d" % s) for s in range(4)]
                xnT = sb("xnT", [128, KC, 512], BF16, es)
                b_xnT = Buf("xnT")
                hidT = sb("hidT", [128, FC, 512], BF16, es)
                b_hidT = Buf("hidT")
                NSL = 3
                slots = [sb("wslot%d" % i, [128, 16 * 512], BF16, es) for i in range(NSL)]
                b_slot = [Buf("slot%d" % i) for i in range(NSL)]
                xtok = sb("xtok", [128, D], BF16, es)
                b_xtok = Buf("xtok")
                xtok2 = sb("xtok2", [128, D], BF16, es)
                xtoks, b_xtoks = [xtok, xtok2], [b_xtok, Buf("xtok2")]
                b_ssp, b_eps = [Buf("ss0"), Buf("ss1")], Buf("eps")
                ss = sb("ss", [128, 8], F32, es)
                b_ss = Buf("ss")
                sgt = [sb("sg%d" % i, [128, 512], F32, es) for i in range(2)]
                b_sg = [Buf() for _ in range(2)]
                if not first:
                    acc = sb("acc", [128, 4, 512], F32, es)
                    b_acc = [Buf() for _ in range(4)]
                    tmp = sb("tmpf", [128, 512], F32, es)
                    b_tmp = Buf("tmp")
                if not last:
                    stg = [sb("stg%d" % i, [128, 4, 512], BF16, es) for i in range(2)]
                    b_stg = [Buf() for _ in range(2)]
                    vst = [sb("vst%d" % i, [128, 512], BF16, es) for i in range(2)]
                    b_vst = [Buf() for _ in range(2)]
                else:
                    gbc = sb("gbc", [128, D], F32, es)
                    b_gbc = Buf("gbc")
                st = {"slot": 0, "bank": 0, "sg": 0, "stg": 0, "vst": 0}
                if last:
                    tgt = len(conv_tasks)
                elif first:
                    tgt = sum(1 for t in conv_tasks if t[1] == 0)
                else:
                    tgt = len(conv_tasks)
                todo = max(0, tgt - cvs["pos"])
                n_pts = NTG * (30 if first else 60)
                tp_per = -(-todo // max(1, n_pts - 20)) if todo > 0 else 0
                if todo > 0:
                    cv_open(es, 3)

                n_win0 = sum(1 for t in conv_tasks if t[1] == 0 and t[0] in WNAMES[:4])
                cur = {"tg": 0}

                def tp_pump():
                    if tp_per > 0 and cvs["pos"] < tgt:
                        k_ = tp_per
                        if first and cur["tg"] == 0 and cvs["pos"] < n_win0:
                            k_ = 12
                        pump(min(k_, tgt - cvs["pos"]), [pool, dve] if (first and cur["tg"] == 0) else [pool])

                def load_panel(W, bW, k0, kn, c0, C):
                    i = st["slot"] % NSL
                    st["slot"] += 1
                    view = slots[i][:, 0:kn * C].rearrange("p (k c) -> p k c", c=C)
                    src_ap = W[k0 * 128:(k0 + kn) * 128, c0:c0 + C].rearrange("(k p) c -> p k c", p=128)
                    dma_sp(view, src_ap, [bW], [b_slot[i]])
                    return view, b_slot[i]

                def rstd_for(s, par=0):
                    c0 = 0 if par == 0 else 5
                    xt, bxt, bs_ = xtoks[par], b_xtoks[par], b_ssp[par]
                    sc.emit(act, lambda e, s=s, xt=xt, c0=c0: e.activation(out=xt[:], in_=h[:, s, :], func=AF.Square,
                                                                         accum_out=ss[:, c0:c0 + 1]), [b_h[s]], [bxt, bs_])
                    sc.emit(act, lambda e, c0=c0: e.activation(out=ss[:, c0 + 1:c0 + 2], in_=ss[:, c0:c0 + 1], func=AF.Sqrt,
                                                               scale=1.0 / D, bias=ss[:, 4:5]), [bs_, b_eps], [bs_])
                    sc.emit(dve, lambda e, c0=c0: e.reciprocal(out=ss[:, c0 + 2:c0 + 3], in_=ss[:, c0 + 1:c0 + 2]),
                            [bs_], [bs_])
                    return c0 + 2

                sc.emit(dve, lambda e: e.memset(ss[:, 4:5], EPS), [], [b_eps])

                def norm_prep(s):
                    par = s % 2
                    rc = rstd_for(s, par)
                    sc.emit(dve, lambda e, s=s, xt=xtoks[par], rc=rc: e.tensor_scalar(
                        out=xt[:], in0=h[:, s, :], scalar1=ss[:, rc:rc + 1], scalar2=None, op0=ALU.mult),
                        [b_h[s], b_ssp[par]], [b_xtoks[par]])

                def norm_transpose(gi):
                    norm_prep(0)
                    for s in range(4):
                        if s + 1 < 4:
                            norm_prep(s + 1)
                        xt, bxt = xtoks[s % 2], b_xtoks[s % 2]
                        for j4 in range(4):
                            bk = 6 + (j4 % 2)
                            pv = banks[bk][:].bitcast(BF16)
                            for jj in range(4):
                                kc_ = j4 * 4 + jj
                                sc.emit(pe, lambda e, pv=pv, jj=jj, kc_=kc_, xt=xt: e.transpose(
                                    pv[:, jj * 128:(jj + 1) * 128], xt[:, kc_ * 128:(kc_ + 1) * 128], ident[:]),
                                    [bxt, b_const], [b_bank[bk]])
                            for jj in range(4):
                                kc_ = j4 * 4 + jj
                                if jj % 2 == 0:
                                    sc.emit(act, lambda e, pv=pv, jj=jj, kc_=kc_, s=s: e.activation(
                                        out=xnT[:, kc_, s * 128:(s + 1) * 128], in_=pv[:, jj * 128:(jj + 1) * 128],
                                        func=AF.Copy, scale=gT[:, gi, kc_:kc_ + 1]),
                                        [b_bank[bk], b_const], [b_xnT])
                                else:
                                    sc.emit(dve, lambda e, pv=pv, jj=jj, kc_=kc_, s=s: e.tensor_scalar(
                                        out=xnT[:, kc_, s * 128:(s + 1) * 128], in0=pv[:, jj * 128:(jj + 1) * 128],
                                        scalar1=gT[:, gi, kc_:kc_ + 1], scalar2=None, op0=ALU.mult),
                                        [b_bank[bk], b_const], [b_xnT])

                def mm_tm(xT, bxT, kct, W, bW, ncols, scale, k_off=0):
                    kp = []
                    k0 = 0
                    while k0 < kct:
                        kn = min(16, kct - k0)
                        kp.append((k0, kn))
                        k0 += kn
                    for ci, c0 in enumerate(range(0, ncols, 512)):
                        tp_pump()
                        for (k0, kn) in kp:
                            pw, bpw = load_panel(W, bW, k0, kn, c0, 512)
                            for s in range(4):
                                bk = (ci % 2) * 4 + s
                                for k in range(kn):
                                    kk = k0 + k
                                    mm(banks[bk][:], xT[:, k_off + kk, s * 128:(s + 1) * 128], pw[:, k, :],
                                       kk == 0, kk == kct - 1, [bpw, bxT], [b_bank[bk]])
                        for s in range(4):
                            bk = (ci % 2) * 4 + s
                            sc.emit(dve, lambda e, s=s, c0=c0, bk=bk: e.scalar_tensor_tensor(
                                out=h[:, s, c0:c0 + 512], in0=banks[bk][:], scalar=float(scale),
                                in1=h[:, s, c0:c0 + 512], op0=ALU.mult, op1=ALU.add),
                                [b_bank[bk], b_h[s]], [b_h[s]])

                def ffn(l_, pre):
                    w1, bw1 = wbf[pre + "_w1"][l_], b_w[pre + "_w1"][l_]
                    w3, bw3 = wbf[pre + "_w3"][l_], b_w[pre + "_w3"][l_]
                    w2, bw2 = wbf[pre + "_w2"][l_], b_w[pre + "_w2"][l_]
                    for c0 in range(0, DFF, 512):
                        tp_pump()
                        p1, bp1 = load_panel(w1, bw1, 0, KC, c0, 512)
                        p3, bp3 = load_panel(w3, bw3, 0, KC, c0, 512)
                        for f in range(4):
                            fc = c0 // 128 + f
                            ba = st["bank"] % 2
                            st["bank"] += 1
                            b1, b3 = ba, 2 + ba
                            for k in range(KC):
                                mm(banks[b1][:], p1[:, k, f * 128:(f + 1) * 128], xnT[:, k, :], k == 0, k == KC - 1,
                                   [bp1, b_xnT], [b_bank[b1]])
                            for k in range(KC):
                                mm(banks[b3][:], p3[:, k, f * 128:(f + 1) * 128], xnT[:, k, :], k == 0, k == KC - 1,
                                   [bp3, b_xnT], [b_bank[b3]])
                            si = st["sg"] % 2
                            st["sg"] += 1
                            sc.emit(act, lambda e, si=si, b1=b1: e.activation(out=sgt[si][:], in_=banks[b1][:], func=AF.Silu),
                                    [b_bank[b1]], [b_sg[si]])
                            sc.emit(dve, lambda e, si=si, b3=b3, fc=fc: e.tensor_tensor(
                                out=hidT[:, fc, :], in0=banks[b3][:], in1=sgt[si][:], op=ALU.mult),
                                [b_bank[b3], b_sg[si]], [b_hidT])
                    mm_tm(hidT, b_hidT, FC, w2, bw2, D, 0.5)

                def inproj_a(l_, tg):
                    W, bW = wbf["w_in"][l_], b_w["w_in"][l_]
                    for (ocol, row, width) in FM_SEGS:
                        tp_pump()
                        pw, bpw = load_panel(W, bW, 0, KC, ocol, width)
                        si = st["stg"] % 2
                        st["stg"] += 1
                        nf = width // 128
                        for f in range(nf):
                            bk = st["bank"] % 4
                            st["bank"] += 1
                            for k in range(KC):
                                mm(banks[bk][:], pw[:, k, f * 128:(f + 1) * 128], xnT[:, k, :], k == 0, k == KC - 1,
                                   [bpw, b_xnT], [b_bank[bk]])
                            if f % 2 == 0:
                                sc.emit(act, lambda e, si=si, f=f, bk=bk: e.copy(out=stg[si][:, f, :], in_=banks[bk][:]),
                                        [b_bank[bk]], [b_stg[si]])
                            else:
                                sc.emit(dve, lambda e, si=si, f=f, bk=bk: e.tensor_copy(out=stg[si][:, f, :], in_=banks[bk][:]),
                                        [b_bank[bk]], [b_stg[si]])
                        dst = featT[row:row + width, tg * 512:(tg + 1) * 512].rearrange("(f p) t -> p f t", p=128)
                        dma_pool(dst, stg[si][:, 0:nf, :], [b_stg[si]], [b_featT])
                    for (ocol, vo, width) in [(O_VA, 0, 512), (O_VB, 512, 512), (O_VC, 1024, 128)]:
                        pw, bpw = load_panel(W, bW, 0, KC, ocol, width)
                        for s in range(4):
                            bk = 4 + s
                            for k in range(KC):
                                mm(banks[bk][:, 0:width], xnT[:, k, s * 128:(s + 1) * 128], pw[:, k, :], k == 0, k == KC - 1,
                                   [bpw, b_xnT], [b_bank[bk]])
                            vi = st["vst"] % 2
                            st["vst"] += 1
                            sc.emit(act, lambda e, vi=vi, bk=bk, width=width: e.copy(
                                out=vst[vi][:, 0:width], in_=banks[bk][:, 0:width]), [b_bank[bk]], [b_vst[vi]])
                            r0 = tg * 512 + s * 128
                            dma_pool(vscr[r0:r0 + 128, vo:vo + width], vst[vi][:, 0:width], [b_vst[vi]], [b_vscr])

                def mixer_post(l_, tg):
                    W, bW = wbf["w_in"][l_], b_w["w_in"][l_]
                    WB, bWB = wbf["w_branch"][l_], b_w["w_branch"][l_]
                    src_ap = yT[:, tg * 512:(tg + 1) * 512].rearrange("(k p) t -> p k t", p=128)
                    dma_pool(hidT[:, 0:16, :], src_ap, [b_yT], [b_hidT])
                    for c0 in range(0, D, 512):
                        for n in range(4):
                            tp_pump()
                            pw, bpw = load_panel(W, bW, 0, KC, O_GATE + n * D + c0, 512)
                            bp, bbp = load_panel(WB, bWB, n * 4, 4, c0, 512)
                            for f in range(4):
                                fc = c0 // 128 + f
                                ba = st["bank"] % 2
                                st["bank"] += 1
                                bg_, bb_ = ba, 2 + ba
                                for k in range(KC):
                                    mm(banks[bg_][:], pw[:, k, f * 128:(f + 1) * 128], xnT[:, k, :], k == 0, k == KC - 1,
                                       [bpw, b_xnT], [b_bank[bg_]])
                                for k in range(4):
                                    mm(banks[bb_][:], bp[:, k, f * 128:(f + 1) * 128], hidT[:, n * 4 + k, :],
                                       k == 0, k == 3, [bbp, b_hidT], [b_bank[bb_]])
                                si = st["sg"] % 2
                                st["sg"] += 1
                                sc.emit(act, lambda e, si=si, bg_=bg_: e.activation(out=sgt[si][:], in_=banks[bg_][:],
                                                                                   func=AF.Sigmoid),
                                        [b_bank[bg_]], [b_sg[si]])
                                if n == 0:
                                    sc.emit(dve, lambda e, si=si, bb_=bb_, f=f: e.tensor_tensor(
                                        out=acc[:, f, :], in0=banks[bb_][:], in1=sgt[si][:], op=ALU.mult),
                                        [b_bank[bb_], b_sg[si]], [b_acc[f]])
                                else:
                                    sc.emit(dve, lambda e, si=si, bb_=bb_: e.tensor_tensor(
                                        out=tmp[:], in0=banks[bb_][:], in1=sgt[si][:], op=ALU.mult),
                                        [b_bank[bb_], b_sg[si]], [b_tmp])
                                    if n < 3:
                                        sc.emit(pool, lambda e, f=f: e.tensor_tensor(out=acc[:, f, :], in0=acc[:, f, :],
                                                                                      in1=tmp[:], op=ALU.add),
                                                [b_acc[f], b_tmp], [b_acc[f]])
                                    else:
                                        sc.emit(pool, lambda e, f=f, fc=fc: e.tensor_tensor(
                                            out=hidT[:, 16 + fc, :], in0=acc[:, f, :], in1=tmp[:], op=ALU.add),
                                            [b_acc[f], b_tmp], [b_hidT])
                    mm_tm(hidT, b_hidT, KC, wbf["w_out"][l_], b_w["w_out"][l_], D, 1.0, k_off=16)

                srcd, bsrc = (x_in, None) if first else (hbuf, b_hbuf)

                def load_h(tg2):
                    for s in range(4):
                        dma_pool(h[:, s, :], srcd[tg2 * 512 + s * 128:tg2 * 512 + (s + 1) * 128, :],
                                 [bsrc] if bsrc else [], [b_h[s]])

                load_h(0)
                for tg in range(NTG):
                    cur["tg"] = tg
                    r0 = tg * 512
                    if last and tg > 0:
                        load_h(tg)
                    if not first:
                        norm_transpose((l - 1) * 3 + 1)
                        mixer_post(l - 1, tg)
                        norm_transpose((l - 1) * 3 + 2)
                        ffn(l - 1, "ffn2")
                    if not last:
                        norm_transpose(l * 3 + 0)
                        ffn(l, "ffn1")
                        for s in range(4):
                            dma_pool(hbuf[r0 + s * 128:r0 + (s + 1) * 128, :], h[:, s, :], [b_h[s]], [b_hbuf])
                        norm_transpose(l * 3 + 1)
                        if first and tg == 0 and cvs["pos"] < n_win0:
                            pump(n_win0 - cvs["pos"], [pool, dve])
                            cv_flush()
                        if tg + 1 < NTG:
                            load_h(tg + 1)
                        inproj_a(l, tg)
                    else:
                        if tg == 0:
                            dma_pool(gbc[:], fnorm[0, :].partition_broadcast(128), [], [b_gbc])
                        for s in range(4):
                            rc = rstd_for(s, s % 2)
                            sc.emit(dve, lambda e, s=s, rc=rc: e.scalar_tensor_tensor(
                                out=h[:, s, :], in0=h[:, s, :], scalar=ss[:, rc:rc + 1], in1=gbc[:],
                                op0=ALU.mult, op1=ALU.mult), [b_h[s], b_ssp[s % 2], b_gbc], [b_h[s]])
                            dma_pool(out_d[r0 + s * 128:r0 + (s + 1) * 128, :], h[:, s, :], [b_h[s]], [b_out])
                if todo > 0:
                    if cvs["pos"] < tgt:
                        pump(tgt - cvs["pos"], [pool, dve])
                    cv_flush()
            sc.barrier()

        def attention_phase(l):
            def load_feat(tile, buf, row, nrows=128, p0=0):
                dma_pool(tile[p0:p0 + nrows, :], featT[row:row + nrows, :], [b_featT], [buf])

            with ExitStack() as es:
                cvbs = [[sb("cvb%d_%d" % (j, i), [128, S], BF16, es) for i in range(3)] for j in range(2)]
                b_cvbs = [[Buf() for _ in range(3)] for _ in range(2)]
                cus = [sb("cu%d" % j, [128, S + 2], F32, es) for j in range(2)]
                cys = [sb("cy%d" % j, [128, S], F32, es) for j in range(2)]
                cyos = [sb("cyo%d" % j, [128, S], BF16, es) for j in range(2)]
                b_cus, b_cys, b_cyos = [Buf(), Buf()], [Buf(), Buf()], [Buf(), Buf()]
                cws = [sb("cw%d" % j, [128, 4], F32, es) for j in range(2)]
                b_cws = [Buf(), Buf()]

                def conv_load(ch):
                    j = ch % 2
                    load_feat(cvbs[j][0], b_cvbs[j][0], R_BG + ch * 128)
                    load_feat(cvbs[j][1], b_cvbs[j][1], R_CG + ch * 128)
                    load_feat(cvbs[j][2], b_cvbs[j][2], R_HD + ch * 128)
                    for k in range(3):
                        dma_pool(cws[j][:, k:k + 1], conv_w[l, k, ch * 128:(ch + 1) * 128].rearrange("(c o) -> c o", o=1),
                                 [], [b_cws[j]])

                conv_load(0)
                for ch in range(4):
                    if ch + 1 < 4:
                        conv_load(ch + 1)
                    j = ch % 2
                    cvb, b_cvb, cu, cy, cyo, cw = cvbs[j], b_cvbs[j], cus[j], cys[j], cyos[j], cws[j]
                    b_cu, b_cy, b_cyo, b_cw = b_cus[j], b_cys[j], b_cyos[j], b_cws[j]
                    sc.emit(dve, lambda e, cu=cu: e.memset(cu[:, 0:2], 0.0), [], [b_cu])
                    sc.emit(dve, lambda e, cu=cu, cvb=cvb: e.tensor_tensor(out=cu[:, 2:S + 2], in0=cvb[1][:], in1=cvb[2][:],
                                                                           op=ALU.mult), [b_cvb[1], b_cvb[2]], [b_cu])
                    sc.emit(dve, lambda e, cu=cu, cy=cy, cw=cw: e.tensor_scalar(
                        out=cy[:], in0=cu[:, 0:S], scalar1=cw[:, 0:1], scalar2=None, op0=ALU.mult), [b_cu, b_cw], [b_cy])
                    sc.emit(dve, lambda e, cu=cu, cy=cy, cw=cw: e.scalar_tensor_tensor(
                        out=cy[:], in0=cu[:, 1:S + 1], scalar=cw[:, 1:2], in1=cy[:], op0=ALU.mult, op1=ALU.add),
                        [b_cu, b_cw, b_cy], [b_cy])
                    sc.emit(dve, lambda e, cu=cu, cy=cy, cw=cw: e.scalar_tensor_tensor(
                        out=cy[:], in0=cu[:, 2:S + 2], scalar=cw[:, 2:3], in1=cy[:], op0=ALU.mult, op1=ALU.add),
                        [b_cu, b_cw, b_cy], [b_cy])
                    sc.emit(dve, lambda e, cy=cy, cyo=cyo, cvb=cvb: e.tensor_tensor(out=cyo[:], in0=cy[:], in1=cvb[0][:],
                                                                                    op=ALU.mult), [b_cy, b_cvb[0]], [b_cyo])
                    dma_pool(yT[1536 + ch * 128:1536 + (ch + 1) * 128, :], cyo[:], [b_cyo], [b_yT])
            sc.barrier()

            with ExitStack() as es:
                b_ac = Buf("attconst")
                cs = {}
                for k_, shape, dt in [("c_mstrict", [128, 128], BF16), ("c_mincl", [128, 128], BF16),
                                      ("c_mpair", [128, 256], BF16), ("c_negu", [128, 128], BF16),
                                      ("c_ind", [128, 160], BF16), ("c_negep", [128, 4096], BF16),
                                      ("c_onesp", [128, 256], BF16), ("c_neg8i", [128, 128], BF16),
                                      ("c_ones128", [128, 128], BF16), ("c_pastneg", [128, 512], F32),
                                      ("c_negbig", [128, 512], F32)]:
                    t_ = sb(k_, shape, dt, es)
                    cs[k_] = t_
                    dma_pool(t_[:], cin[k_][:], [], [b_ac])
                cv_open(es, 3)
                n_iter_pts = 3 * 4 * 2 * NTG
                att_tgt = sum(1 for t in conv_tasks if t[1] <= l or (t[1] == l + 1 and t[0] in WNAMES[:4]))
                remaining = max(0, att_tgt - cvs["pos"])
                per_pt = 0
                w_tot = 2 * 4 * sum(4 * g_ + 4 for g_ in range(NTG)) * 1.6 + 0.2 * 64
                quota = {"acc": 0.0}

                def wpump(w):
                    if remaining <= 0:
                        return
                    quota["acc"] += 1.15 * remaining * w / w_tot
                    k_ = int(quota["acc"])
                    if k_ > 0:
                        quota["acc"] -= k_
                        pump(k_, [pool])
                qzs = [[sb("qz%d_%d" % (i, hh), [128, S], BF16, es) for hh in range(2)] for i in range(2)]
                kTs = [sb("kT%d" % i, [128, S], BF16, es) for i in range(2)]
                vzs = [[sb("vz%d_%d" % (i, hh), [128, NQT, 128], BF16, es) for hh in range(2)] for i in range(2)]
                b_qs, b_ks, b_vs = [Buf(), Buf()], [Buf(), Buf()], [Buf(), Buf()]
                for i in range(2):
                    for hh in range(2):
                        o0 = 64 * (1 - hh)
                        sc.emit(pool, lambda e, t=qzs[i][hh], o0=o0: e.memset(t[o0:o0 + 64, :], 0.0), [], [b_qs[i]])
                        sc.emit(pool, lambda e, t=vzs[i][hh], o0=o0: e.memset(t[:, :, o0:o0 + 64], 0.0), [], [b_vs[i]])
                qaugs = [sb("qaug%d" % i, [128, S], BF16, es) for i in range(2)]
                kaugs = [sb("kaug%d" % i, [128, S], BF16, es) for i in range(2)]
                b_qaugs, b_kaugs = [Buf(), Buf()], [Buf(), Buf()]
                for i in range(2):
                    sc.emit(pool, lambda e, t=qaugs[i]: e.memset(t[:], 0.0), [], [b_qaugs[i]])
                    sc.emit(pool, lambda e, t=kaugs[i]: e.memset(t[:], 0.0), [], [b_kaugs[i]])
                NSP = 4
                sp_sb = [sb("sp_sb%d" % i, [128, 512], BF16, es) for i in range(NSP)]
                b_sps = [Buf() for _ in range(NSP)]
                carry_bf = [sb("carry%d" % i, [128, 512], BF16, es) for i in range(2)]
                b_carry = [Buf() for _ in range(2)]
                zero_bf = sb("zero_bf", [128, 512], BF16, es)
                sc.emit(pool, lambda e: e.memset(zero_bf[:], 0.0), [], [b_ac])
                e_sb = [sb("e_sb%d" % i, [128, 512], F32, es) for i in range(2)]
                b_e = [Buf() for _ in range(2)]
                NA = 4
                a_sb = [sb("a_sb%d" % i, [128, 512], BF16, es) for i in range(NA)]
                b_a = [Buf() for _ in range(NA)]
                tot_sb = sb("tot_sb", [128, 512], BF16, es)
                b_tot = Buf("tot")
                ost = [sb("ost%d" % i, [128, 512], BF16, es) for i in range(2)]
                b_ost = [Buf() for _ in range(2)]
                rden = sb("rden", [128, 512], F32, es)
                b_rden = Buf("rden")
                esink = sb("esink", [128, 2], F32, es)
                b_esink = Buf("esink")
                kmeans = [sb("kmean%d" % i, [128, 16], F32, es) for i in range(2)]
                kmTs = [sb("kmT%d" % i, [128, 16], BF16, es) for i in range(2)]
                b_kms = [Buf(), Buf()]
                gm = sb("gm", [128, 32, 16], F32, es)
                top8 = sb("top8", [128, 32, 8], F32, es)
                selb = sb("selb", [128, 32, 16], BF16, es)
                lt = sb("lt", [128, 32, 16], F32, es)
                b_gm, b_top8, b_selb, b_lt = Buf("gm"), Buf("top8"), Buf("selb"), Buf("lt")
                ctr = {"z": 0, "g": 0, "e": 0, "a": 0, "o": 0, "ost": 0, "sp": 0, "mz": 0}

                def pipeline(items, stage_c, stage_d, la=2, hooks=None):
                    n_ = len(items)
                    for i in range(n_ + la):
                        if hooks and i in hooks:
                            hooks[i]()
                        if i < n_:
                            stage_c(items[i])
                        if i - la >= 0:
                            stage_d(items[i - la])

                def qk_cols(g, ks):
                    col0 = max(0, ks - 4 * g) * 128
                    return col0, 512 - col0

                pair_jobs = [("sb", p) for p in range(4)] + [("moba", p) for p in range(4)] + [("swa", p) for p in range(4)]

                def load_pair(ji):
                    kind, p = pair_jobs[ji]
                    i = ji % 2
                    if kind == "sb":
                        qrow, krow, vcol = R_QB + p * 128, R_KB + p * 128, 512 + p * 128
                    elif kind == "moba":
                        qrow, krow, vcol = R_QA + p * 128, R_KA + p * 128, p * 128
                    else:
                        qrow, vcol = R_QC + p * 128, 1024 + (p // 2) * 64
                    for hh in range(2):
                        dma_pool(qzs[i][hh][64 * hh:64 * hh + 64, :], featT[qrow + 64 * hh:qrow + 64 * hh + 64, :],
                                 [b_featT], [b_qs[i]])
                    if kind == "swa":
                        kr = R_KC + (p // 2) * 64
                        dma_pool(kTs[i][0:64, :], featT[kr:kr + 64, :], [b_featT], [b_ks[i]])
                        dma_pool(kTs[i][64:128, :], featT[kr:kr + 64, :], [b_featT], [b_ks[i]])
                    else:
                        dma_pool(kTs[i][:, :], featT[krow:krow + 128, :], [b_featT], [b_ks[i]])
                    for hh in range(2):
                        vc = vcol if kind == "swa" else vcol + 64 * hh
                        src_ap = vscr[:, vc:vc + 64].rearrange("(t p) c -> p t c", p=128)
                        dma_pool(vzs[i][hh][:, :, 64 * hh:64 * hh + 64], src_ap, [b_vscr], [b_vs[i]])

                def evac_norm_store(nb_, db_, base, row, g, add_sink):
                    if add_sink:
                        sc.emit(act, lambda e: e.activation(
                            out=rden[base:base + 64, :], in_=banks[db_][base:base + 64, :], func=AF.Ln,
                            bias=esink[base:base + 64, 1:2], scale=1.0), [b_bank[db_], b_esink], [b_rden])
                        sc.emit(act, lambda e: e.activation(
                            out=rden[base:base + 64, :], in_=rden[base:base + 64, :], func=AF.Exp, scale=-1.0),
                            [b_rden], [b_rden])
                    else:
                        sc.emit(dve, lambda e: e.reciprocal(out=rden[base:base + 64, :], in_=banks[db_][base:base + 64, :]),
                                [b_bank[db_]], [b_rden])
                    si = ctr["ost"] % 2
                    ctr["ost"] += 1
                    sc.emit(dve, lambda e: e.tensor_tensor(
                        out=ost[si][base:base + 64, :], in0=banks[nb_][base:base + 64, :],
                        in1=rden[base:base + 64, :], op=ALU.mult), [b_bank[nb_], b_rden], [b_ost[si]])
                    dma_pool(yT[row + base:row + base + 64, g * 512:(g + 1) * 512],
                             ost[si][base:base + 64, :], [b_ost[si]], [b_yT])

                load_pair(0)
                for ji, (kind, p) in enumerate(pair_jobs):
                    if ji + 1 < len(pair_jobs):
                        load_pair(ji + 1)
                    pi = ji % 2
                    kT = kTs[pi]
                    b_q, b_k, b_v = b_qs[pi], b_ks[pi], b_vs[pi]

                    if kind == "sb":
                        for g in range(NTG):
                            kmax = 4 * g + 3
                            ob, tb = 5, 4
                            for hh in range(2):
                                base = 64 * hh
                                qz, vz = qzs[pi][hh], vzs[pi][hh]
                                info = {}
                                mm(banks[tb][:], cs["c_ones128"][:], zero_bf[:], True, False, [b_ac], [b_bank[tb]])
                                mm(banks[ob][:], cs["c_ones128"][:], zero_bf[:], True, False, [b_ac], [b_bank[ob]])

                                def s1(ks, qz=qz, kT=kT, g=g, info=info):
                                    col0, n = qk_cols(g, ks)
                                    diag = ks >= 4 * g
                                    bk = ctr["z"] % 4
                                    ctr["z"] += 1
                                    mm(banks[bk][:, col0:512], kT[:, ks * 128:(ks + 1) * 128],
                                       qz[:, g * 512 + col0:(g + 1) * 512], True, False, [b_k, b_q], [b_bank[bk]])
                                    if diag:
                                        mm(banks[bk][:, col0:col0 + 128], ident[:], cs["c_mstrict"][:], False, False,
                                           [b_const, b_ac], [b_bank[bk]])
                                    ei = ctr["e"] % 2
                                    ctr["e"] += 1
                                    si_ = ctr["sp"] % NSP
                                    ctr["sp"] += 1
                                    info[ks] = (bk, col0, si_)
                                    sc.emit(act, lambda e, bk=bk, ei=ei, col0=col0: e.activation(
                                        out=e_sb[ei][:, col0:512], in_=banks[bk][:, col0:512], func=AF.Exp, scale=0.125),
                                        [b_bank[bk]], [b_e[ei]])
                                    sc.emit(act, lambda e, ei=ei, si_=si_, col0=col0: e.activation(
                                        out=sp_sb[si_][:, col0:512], in_=e_sb[ei][:, col0:512], func=AF.Ln,
                                        scale=1.0, bias=1.0), [b_e[ei]], [b_sps[si_]])

                                def s2(ks, kmax=kmax, info=info, tb=tb):
                                    bk, col0, si_ = info[ks]
                                    first = ks == kmax
                                    mm(banks[bk][:, col0:512], cs["c_negu"][:], sp_sb[si_][:, col0:512], False, first,
                                       [b_sps[si_], b_ac], [b_bank[bk]])
                                    if not first:
                                        ci_ = (ks + 1) % 2
                                        mm(banks[bk][:, col0:512], cs["c_neg8i"][:], carry_bf[ci_][:, col0:512], False, True,
                                           [b_carry[ci_], b_ac], [b_bank[bk]])
                                    if ks > 0:
                                        mm(banks[tb][:, col0:512], cs["c_ones128"][:], sp_sb[si_][:, col0:512], False, False,
                                           [b_sps[si_], b_ac], [b_bank[tb]])
                                        co_ = ks % 2
                                        sc.emit(dve, lambda e, co_=co_, tb=tb: e.tensor_copy(out=carry_bf[co_][:], in_=banks[tb][:]),
                                                [b_bank[tb]], [b_carry[co_]])
                                    ai = ctr["a"] % NA
                                    ctr["a"] += 1
                                    info[ks] = (bk, col0, si_, ai)
                                    sc.emit(act, lambda e, bk=bk, ai=ai, col0=col0: e.activation(
                                        out=a_sb[ai][:, col0:512], in_=banks[bk][:, col0:512], func=AF.Exp, scale=0.125),
                                        [b_bank[bk]], [b_a[ai]])

                                def s3(ks, info=info, vz=vz, ob=ob):
                                    bk, col0, si_, ai = info[ks]
                                    mm(banks[ob][:, col0:512], vz[:, ks, :], a_sb[ai][:, col0:512], False, ks == 0,
                                       [b_v, b_a[ai]], [b_bank[ob]])

                                order = list(range(kmax, -1, -1))
                                n_ = len(order)
                                for i in range(n_ + 2):
                                    if i < n_:
                                        s1(order[i])
                                    if 0 <= i - 1 < n_:
                                        s2(order[i - 1])
                                    if 0 <= i - 2 < n_:
                                        s3(order[i - 2])
                                si = ctr["ost"] % 2
                                ctr["ost"] += 1
                                sc.emit(dve, lambda e, si=si, ob=ob, base=base: e.tensor_copy(
                                    out=ost[si][base:base + 64, :], in_=banks[ob][base:base + 64, :]),
                                    [b_bank[ob]], [b_ost[si]])
                                r_ = 512 + p * 128 + base
                                dma_pool(yT[r_:r_ + 64, g * 512:(g + 1) * 512], ost[si][base:base + 64, :],
                                         [b_ost[si]], [b_yT])
                                wpump(4 * g + 4)

                    elif kind == "moba":
                        nq16 = NQT * 16
                        GB = 3
                        moba_heads = [(ji2, p2, hh2) for ji2, (k2, p2) in enumerate(pair_jobs) if k2 == "moba" for hh2 in range(2)]

                        def prep_a(m):
                            ji2, p2, hh2 = moba_heads[m]
                            pi2 = ji2 % 2
                            kT2, qz2 = kTs[pi2], qzs[pi2][hh2]
                            kmean, kmT, b_km = kmeans[p2 % 2], kmTs[p2 % 2], b_kms[p2 % 2]
                            if hh2 == 0:
                                sc.emit(dve, lambda e, kT2=kT2, kmean=kmean: e.tensor_reduce(
                                    out=kmean[:, 0:NBLK], in_=kT2[:, :].rearrange("p (n j) -> p n j", j=256),
                                    axis=AX.X, op=ALU.add), [b_ks[pi2]], [b_km])
                                if NBLK < 16:
                                    sc.emit(dve, lambda e, kmT=kmT: e.memset(kmT[:, NBLK:16], 0.0), [], [b_km])
                                sc.emit(dve, lambda e, kmT=kmT, kmean=kmean: e.tensor_scalar(
                                    out=kmT[:, 0:NBLK], in0=kmean[:, 0:NBLK], scalar1=1.0 / 256.0,
                                    scalar2=None, op0=ALU.mult), [b_km], [b_km])
                            hd2 = 2 * p2 + hh2
                            qaug, kaug, b_qaug, b_kaug = qaugs[hh2], kaugs[hh2], b_qaugs[hh2], b_kaugs[hh2]
                            dma_pool(qaug[16:20, :], cin["c_qaug_a"][hd2], [], [b_qaug])
                            dma_pool(kaug[0:20, :], cin["c_kaug_a"][hd2], [], [b_kaug])
                            for tq in range(NQT):
                                mm(banks[GB][:, tq * 16:(tq + 1) * 16], qz2[:, tq * 128:(tq + 1) * 128],
                                   kmT[:, :], True, True, [b_qs[pi2], b_km], [b_bank[GB]])
                            gflat = gm[:].rearrange("p a b -> p (a b)")
                            sc.emit(dve, lambda e: e.tensor_tensor(out=gflat[:, 0:nq16], in0=banks[GB][:, 0:nq16],
                                                                   in1=cs["c_pastneg"][:, 0:nq16], op=ALU.add),
                                    [b_bank[GB], b_ac], [b_gm])
                            for tq in range(NQT):
                                sc.emit(dve, lambda e, tq=tq: e.max(out=top8[:, tq, :], in_=gm[:, tq, :]), [b_gm], [b_top8])
                            sc.emit(dve, lambda e: e.tensor_tensor(
                                out=lt[:, 0:NQT, :], in0=gm[:, 0:NQT, :],
                                in1=top8[:, 0:NQT, 2:3].broadcast_to([128, NQT, 16]), op=ALU.is_lt),
                                [b_gm, b_top8], [b_lt])
                            sc.emit(dve, lambda e: e.tensor_tensor(
                                out=selb[:, 0:NQT, :], in0=lt[:, 0:NQT, :],
                                in1=cs["c_negbig"][:, 0:nq16].rearrange("p (a b) -> p a b", b=16), op=ALU.mult),
                                [b_lt, b_ac], [b_selb])

                        def prep_b(m):
                            ji2, p2, hh2 = moba_heads[m]
                            qaug, b_qaug = qaugs[hh2], b_qaugs[hh2]
                            for t8 in range(0, NQT, 8):
                                pv = banks[GB][:].bitcast(BF16)
                                for jj in range(8):
                                    tq = t8 + jj
                                    sc.emit(pe, lambda e, pv=pv, jj=jj, tq=tq: e.transpose(
                                        pv[0:16, jj * 128:(jj + 1) * 128], selb[:, tq, :], ident[:]),
                                        [b_selb, b_const], [b_bank[GB]])
                                sc.emit(act, lambda e, pv=pv, t8=t8, qaug=qaug: e.copy(
                                    out=qaug[0:16, t8 * 128:(t8 + 8) * 128], in_=pv[0:16, 0:1024]),
                                    [b_bank[GB]], [b_qaug])

                        for hh in range(2):
                            m = moba_heads.index((ji, p, hh))
                            if m == 0:
                                prep_a(0)
                                prep_b(0)
                            base = 64 * hh
                            qz, vz = qzs[pi][hh], vzs[pi][hh]
                            qaug, kaug, b_qaug, b_kaug = qaugs[hh], kaugs[hh], b_qaugs[hh], b_kaugs[hh]
                            nbs = {}

                            def mo_c(item, qz=qz, kT=kT, nbs=nbs, qaug=qaug, kaug=kaug, b_qaug=b_qaug, b_kaug=b_kaug):
                                g, ks = item
                                bk = ctr["mz"] % 3
                                ctr["mz"] += 1
                                col0, n = qk_cols(g, ks)
                                diag = ks >= 4 * g
                                mm(banks[bk][:, col0:512], kT[:, ks * 128:(ks + 1) * 128],
                                   qz[:, g * 512 + col0:(g + 1) * 512], True, False,
                                   [b_k, b_q], [b_bank[bk]])
                                mm(banks[bk][:, col0:512], kaug[:, ks * 128:(ks + 1) * 128],
                                   qaug[:, g * 512 + col0:(g + 1) * 512], False, not diag,
                                   [b_kaug, b_qaug], [b_bank[bk]])
                                if diag:
                                    mm(banks[bk][:, col0:col0 + 128], ident[:], cs["c_mincl"][:], False, True,
                                       [b_const, b_ac], [b_bank[bk]])
                                ai = ctr["a"] % NA
                                ctr["a"] += 1
                                nbs[item] = ai
                                sc.emit(act, lambda e, bk=bk, ai=ai, col0=col0: e.activation(
                                    out=a_sb[ai][:, col0:512], in_=banks[bk][:, col0:512], func=AF.Exp, scale=0.125),
                                    [b_bank[bk]], [b_a[ai]])

                            def mo_d(item, base=base, p=p, vz=vz, hh=hh, nbs=nbs):
                                g, ks = item
                                kmax = 4 * g + 3
                                col0, n = qk_cols(g, ks)
                                ai = nbs[item]
                                nb_ = 4 + (g % 2)
                                db_ = 6 + (g % 2)
                                mm(banks[nb_][:, col0:512], vz[:, ks, :], a_sb[ai][:, col0:512],
                                   ks == 0, ks == kmax, [b_v, b_a[ai]], [b_bank[nb_]])
                                mm(banks[db_][:, col0:512], cs["c_onesp"][:, hh * 128:(hh + 1) * 128], a_sb[ai][:, col0:512],
                                   ks == 0, ks == kmax, [b_ac, b_a[ai]], [b_bank[db_]])
                                if ks == kmax:
                                    evac_norm_store(nb_, db_, base, p * 128, g, False)
                                    wpump(0.6 * (4 * g + 4))

                            items = [(g, ks) for g in range(NTG) for ks in range(4 * g + 4)]
                            hooks = None
                            if m + 1 < len(moba_heads):
                                hooks = {len(items) // 2: (lambda m=m: prep_a(m + 1)),
                                         (3 * len(items)) // 4: (lambda m=m: prep_b(m + 1))}
                            pipeline(items, mo_c, mo_d, la=2, hooks=hooks)

                    else:
                        if p == 0:
                            for i_ in range(2):
                                sc.emit(pool, lambda e, t=qaugs[i_]: e.memset(t[0:16, :], 0.0), [], [b_qaugs[i_]])
                        dma_pool(esink[0:64, 0:1], sinks[l, 2 * p:2 * p + 1].partition_broadcast(64), [], [b_esink])
                        dma_pool(esink[64:128, 0:1], sinks[l, 2 * p + 1:2 * p + 2].partition_broadcast(64), [], [b_esink])
                        sc.emit(act, lambda e: e.activation(out=esink[:, 1:2], in_=esink[:, 0:1], func=AF.Exp),
                                [b_esink], [b_esink])
                        for hh in range(2):
                            hd_ = 2 * p + hh
                            base = 64 * hh
                            qz, vz = qzs[pi][hh], vzs[pi][hh]
                            qaug, kaug, b_qaug, b_kaug = qaugs[hh], kaugs[hh], b_qaugs[hh], b_kaugs[hh]
                            dma_pool(qaug[16:20, :], cin["c_qaug_c"][hd_], [], [b_qaug])
                            dma_pool(kaug[16:20, :], cin["c_kaug_c"][hd_], [], [b_kaug])
                            nbs = {}

                            def sw_c(tq, qz=qz, kT=kT, nbs=nbs, qaug=qaug, kaug=kaug, b_qaug=b_qaug, b_kaug=b_kaug):
                                bk = ctr["z"] % 4
                                ctr["z"] += 1
                                c_lo = 0 if tq > 0 else 128
                                qs = slice(tq * 128, (tq + 1) * 128)
                                ksl = [ks for ks in (tq - 1, tq) if ks >= 0]
                                for ks in ksl:
                                    w_ = ks - (tq - 1)
                                    ws = slice(w_ * 128, (w_ + 1) * 128)
                                    mm(banks[bk][:, ws], kT[:, ks * 128:(ks + 1) * 128],
                                       qz[:, qs], True, False, [b_k, b_q], [b_bank[bk]])
                                    mm(banks[bk][:, ws], kaug[:, ks * 128:(ks + 1) * 128],
                                       qaug[:, qs], False, False, [b_kaug, b_qaug], [b_bank[bk]])
                                    mm(banks[bk][:, ws], ident[:], cs["c_mpair"][:, ws], False, True,
                                       [b_const, b_ac], [b_bank[bk]])
                                ai = ctr["a"] % NA
                                ctr["a"] += 1
                                nbs[tq] = ai
                                sc.emit(act, lambda e, bk=bk, ai=ai, c_lo=c_lo: e.activation(
                                    out=a_sb[ai][:, c_lo:256], in_=banks[bk][:, c_lo:256], func=AF.Exp, scale=0.125),
                                    [b_bank[bk]], [b_a[ai]])

                            def sw_d(tq, base=base, p=p, vz=vz, hh=hh, nbs=nbs):
                                g, t4 = tq // 4, tq % 4
                                ai = nbs[tq]
                                nb_ = 4 + (g % 2)
                                db_ = 6 + (g % 2)
                                ksl = [ks for ks in (tq - 1, tq) if ks >= 0]
                                for idx, ks in enumerate(ksl):
                                    w_ = ks - (tq - 1)
                                    mm(banks[nb_][:, t4 * 128:(t4 + 1) * 128], vz[:, ks, :],
                                       a_sb[ai][:, w_ * 128:(w_ + 1) * 128], idx == 0, idx == len(ksl) - 1,
                                       [b_v, b_a[ai]], [b_bank[nb_]])
                                for idx, ks in enumerate(ksl):
                                    w_ = ks - (tq - 1)
                                    mm(banks[db_][:, t4 * 128:(t4 + 1) * 128], cs["c_onesp"][:, hh * 128:(hh + 1) * 128],
                                       a_sb[ai][:, w_ * 128:(w_ + 1) * 128], idx == 0, idx == len(ksl) - 1,
                                       [b_ac, b_a[ai]], [b_bank[db_]])
                                if t4 == 3:
                                    evac_norm_store(nb_, db_, base, 1024 + p * 128, g, True)
                                    wpump(0.2)

                            pipeline(list(range(NQT)), sw_c, sw_d, la=2)
                if cvs["pos"] < att_tgt:
                    pump(att_tgt - cvs["pos"], [dve, pool])
                cv_flush()
            sc.barrier()

        for l in range(L + 1):
            token_phase(first=(l == 0), last=(l == L), l=l)
            if l < L:
                attention_phase(l)

        sc.emit(sp, lambda e: e.nop(), [b_out], [])

        with nc.Block() as block:
            @block.tensor
            def _(e):
                sc.replay(sc.pe, e)

            @block.scalar
            def _(e):
                sc.replay(sc.act, e)

            @block.vector
            def _(e):
                sc.replay(sc.dve, e)

            @block.gpsimd
            def _(e):
                sc.replay(sc.pool, e)

            @block.sync
            def _(e):
                sc.replay(sc.sp, e)
    return nc, consts


def make_in_maps(inputs, consts, L, nb):
    x = np.ascontiguousarray(inputs["x"], dtype=np.float32)
    shared = {}
    for n in WNAMES:
        w = np.asarray(inputs[n], dtype=np.float32)
        shared[n] = np.ascontiguousarray(w.reshape((L,) + WSHAPES[n]))
    for n in ["ffn1_norm", "mix_norm", "ffn2_norm"]:
        shared[n] = np.ascontiguousarray(inputs[n], dtype=np.float32)
    shared["final_norm"] = np.ascontiguousarray(np.asarray(inputs["final_norm"], np.float32).reshape(1, D))
    shared["conv_w"] = np.ascontiguousarray(np.asarray(inputs["conv_w"], np.float32).reshape(L, 3, 512))
    shared["attn_sinks"] = np.ascontiguousarray(inputs["attn_sinks"], dtype=np.float32)
    shared.update(consts)
    return [dict(shared, x=x[b]) for b in range(nb)]


_CACHE = {}


def kernel(**inputs):
    S, L = 4096, 2
    if "prog" not in _CACHE:
        _CACHE["prog"] = build(S, L)
    nc, consts = _CACHE["prog"]
    B = np.asarray(inputs["x"]).shape[0]
    in_maps = make_in_maps(inputs, consts, L, B)
    res = run_bass_kernel_spmd(nc, in_maps, core_ids=list(range(B)))
    return np.stack([np.asarray(r["out"]) for r in res.results], axis=0).astype(np.float32)
```
